# Optimizing a Trainium2 kernel written in Bass

```python
import math
import jax
import jax.numpy as jnp
from jax import lax
import numpy as np

D_MODEL = 1024
BATCH = 8
SEQ = 2048
DEPTH = 4

GRID_W = 64
CTX_LEN = 256

RWKV_WIDTH = D_MODEL // 2
RWKV_HEAD = 64
RWKV_HEADS = RWKV_WIDTH // RWKV_HEAD
DECAY_LORA = 64
ICLR_LORA = 64
GATE_LORA = 128
CONV_WIDTH = D_MODEL - RWKV_WIDTH
CONV_K = 3
LNX_EPS = 64e-5

DIFF_HEADS = 4
DIFF_HEAD = 64
DIFF_VHEAD = 2 * DIFF_HEAD
DIFF_WIDTH = DIFF_HEADS * DIFF_VHEAD
NA_HEADS = 8
NA_HEAD = 64
NA_WIDTH = NA_HEADS * NA_HEAD
NA_MAX_ROWS = 8
NA_COLS = 16
ROPE_BASE = 10000.0
Q_BLOCK = 128
SUBLN_EPS = 1e-5

N_GROUPS = 4
EXPERTS_PER_GROUP = 4
N_EXPERTS = N_GROUPS * EXPERTS_PER_GROUP
TOP_K_IN_GROUP = 2
D_EXPERT = 512
RMS_EPS = 1e-6

N_EVEN = (DEPTH + 1) // 2
N_ODD = DEPTH // 2
EVEN_WIDTHS = (RWKV_WIDTH, RWKV_WIDTH, RWKV_WIDTH, DECAY_LORA, DECAY_LORA, ICLR_LORA, ICLR_LORA, GATE_LORA, CONV_WIDTH, CONV_WIDTH, CONV_WIDTH)
EVEN_PROJ = sum(EVEN_WIDTHS)
ODD_WIDTHS = (DIFF_HEADS * 2 * DIFF_HEAD, DIFF_HEADS * 2 * DIFF_HEAD, DIFF_WIDTH, NA_WIDTH, NA_WIDTH, NA_WIDTH)
ODD_PROJ = sum(ODD_WIDTHS)

kernel_name = 'hybrid_rwkv7_shortconv_diffattn_natten_hmoe'

F32 = jnp.float32


def split_cols(p, widths):
    return jnp.split(p, np.cumsum(widths)[:-1].tolist(), axis=-1)


def rmsnorm(x, g, eps=RMS_EPS):
    xf = x.astype(F32)
    y = xf * lax.rsqrt(jnp.mean(xf * xf, axis=-1, keepdims=True) + eps)
    return (y * g.astype(F32)).astype(x.dtype)


def modulate(x, g, shift, scale):
    return rmsnorm(x, g) * (1.0 + scale) + shift


def ada_mod(cond, w, b):
    return jnp.split(jax.nn.silu(cond) @ w + b, 6, axis=-1)


def heads(t, n_heads, head_dim):
    return t.reshape(t.shape[:-1] + (n_heads, head_dim))


def rwkv7_scan(r, decay, k, v, kk, a, s0, reverse):
    def step(S, inp):
        r_t, w_t, k_t, v_t, kk_t, a_t = inp
        s_kk = jnp.einsum('bhvk,bhk->bhv', S, kk_t)
        S = (S * w_t[:, :, None, :] - s_kk[..., None] * (kk_t * a_t)[:, :, None, :]
             + v_t[..., None] * k_t[:, :, None, :])
        return S, jnp.einsum('bhvk,bhk->bhv', S, r_t)
    xs = tuple(jnp.moveaxis(t, 1, 0) for t in (r, decay, k, v, kk, a))
    s_final, ys = lax.scan(step, s0, xs, reverse=reverse)
    return jnp.moveaxis(ys, 0, 1), s_final


def rwkv7_bidir_scan(r, k, v, wd, ad, w0, w_up, a0, a_up, k_k, k_a, s0):
    hd = lambda t: heads(t, RWKV_HEADS, RWKV_HEAD)
    kk = hd(k * k_k)
    kk = kk * lax.rsqrt(jnp.maximum(jnp.sum(kk * kk, axis=-1, keepdims=True), 1e-24))
    ys, finals = [], []
    for d in range(2):
        w = w0[d] + jnp.tanh(wd[d]) @ w_up[d]
        decay = jnp.exp(-jnp.exp(-jax.nn.softplus(-w) - 0.5))
        a = jax.nn.sigmoid(a0[d] + ad[d] @ a_up[d])
        k_d = k * (1.0 + (a - 1.0) * k_a)
        y, s = rwkv7_scan(hd(r), hd(decay), hd(k_d), hd(v), kk, hd(a), s0[d], reverse=(d == 1))
        ys.append(y)
        finals.append(s)
    return ys[0] + ys[1], (finals[0], finals[1])


def rwkv7_readout(y, r, k, v, gd, g_up, r_k, lnx_w, lnx_b):
    hd = lambda t: heads(t, RWKV_HEADS, RWKV_HEAD)
    B, L = y.shape[:2]
    yc = y - jnp.mean(y, axis=-1, keepdims=True)
    yn = yc * lax.rsqrt(jnp.mean(yc * yc, axis=-1, keepdims=True) + LNX_EPS)
    out = yn.reshape(B, L, RWKV_WIDTH) * lnx_w + lnx_b
    bonus = jnp.sum(hd(r * k) * r_k, axis=-1, keepdims=True) * hd(v)
    out = out + bonus.reshape(B, L, RWKV_WIDTH)
    g = jax.nn.sigmoid(gd) @ g_up
    return out * g


def short_conv(b_gate, c_gate, x_in, conv_w):
    L = x_in.shape[1]
    u = jnp.pad(c_gate * x_in, ((0, 0), (CONV_K // 2, CONV_K // 2), (0, 0)))
    y = sum(u[:, j:j + L] * conv_w[j] for j in range(CONV_K))
    return b_gate * y


def even_mixer(pc, px, w0, w_up, a0, a_up, g_up, k_k, k_a, r_k, lnx_w, lnx_b, conv_w, need_ctx):
    c_parts = split_cols(pc.astype(F32), EVEN_WIDTHS)
    x_parts = split_cols(px.astype(F32), EVEN_WIDTHS)
    B = px.shape[0]
    zeros = jnp.zeros((B, RWKV_HEADS, RWKV_HEAD, RWKV_HEAD), F32)

    def scan_stream(p, s0):
        r, k, v, wd_f, wd_b, ad_f, ad_b = p[:7]
        return rwkv7_bidir_scan(r, k, v, (wd_f, wd_b), (ad_f, ad_b), w0, w_up, a0, a_up, k_k, k_a, s0)

    def finish(p, y, dtype):
        r, k, v, gd = p[0], p[1], p[2], p[7]
        h_a = rwkv7_readout(y, r, k, v, gd, g_up, r_k, lnx_w, lnx_b)
        h_b = short_conv(p[8], p[9], p[10], conv_w)
        return jnp.concatenate([h_a, h_b], axis=-1).astype(dtype)

    y_c, s_ctx = scan_stream(c_parts, (zeros, zeros))
    y_x, _ = scan_stream(x_parts, s_ctx)
    h_x = finish(x_parts, y_x, px.dtype)
    h_c = finish(c_parts, y_c, pc.dtype) if need_ctx else None
    return h_x, h_c


def axial_rope_tables(n_tokens, head_dim):
    n_freq = head_dim // 4
    inv_freq = ROPE_BASE ** (-jnp.arange(n_freq, dtype=F32) / n_freq)
    t = jnp.arange(n_tokens, dtype=jnp.int32)
    pos = jnp.stack([t // GRID_W, t % GRID_W], axis=-1).astype(F32)
    ang = pos[:, :, None] * inv_freq
    return jnp.cos(ang), jnp.sin(ang)


def apply_axial_rope(x, cos, sin):
    shp = x.shape
    xs = x.astype(F32).reshape(shp[:-1] + (2, 2, shp[-1] // 4))
    x1, x2 = xs[..., 0, :], xs[..., 1, :]
    out = jnp.stack([x1 * cos - x2 * sin, x2 * cos + x1 * sin], axis=-2)
    return out.reshape(shp).astype(x.dtype)


def diff_attend(q, k, v, lam):
    s = jnp.einsum('bqhmd,bkhmd->bhmqk', q, k).astype(F32) * (DIFF_HEAD ** -0.5)
    p = jax.nn.softmax(s, axis=-1)
    w = p[:, :, 0] - lam * p[:, :, 1]
    return jnp.einsum('bhqk,bkhe->bqhe', w, v.astype(F32))


def dense_attend(q, k, v):
    s = jnp.einsum('bqhd,bkhd->bhqk', q, k).astype(F32) * (q.shape[-1] ** -0.5)
    return jnp.einsum('bhqk,bkhd->bqhd', jax.nn.softmax(s, axis=-1), v.astype(F32))


def na_latent(q, k, v, k_ctx, v_ctx, rel_bias, rows):
    B, L, H, d = q.shape
    kr = min(NA_MAX_ROWS, rows)
    n_ctx = k_ctx.shape[1]
    scale = d ** -0.5
    k_grid = k.reshape(B, rows, GRID_W, H, d)
    v_grid = v.reshape(B, rows, GRID_W, H, d)
    cols = jnp.arange(GRID_W)
    col_start = jnp.clip(cols - NA_COLS // 2, 0, GRID_W - NA_COLS)
    col_idx = col_start[:, None] + jnp.arange(NA_COLS)[None, :]
    col_off = col_idx - cols[:, None] + (NA_COLS - 1)

    def row_block(args):
        r, q_row = args
        r_start = jnp.clip(r - kr // 2, 0, rows - kr)
        k_win = lax.dynamic_slice_in_dim(k_grid, r_start, kr, axis=1)[:, :, col_idx]
        v_win = lax.dynamic_slice_in_dim(v_grid, r_start, kr, axis=1)[:, :, col_idx]
        row_off = r_start + jnp.arange(kr) - r + (NA_MAX_ROWS - 1)
        bias = rel_bias[:, row_off[None, :, None], col_off[:, None, :]]
        s_loc = jnp.einsum('bchd,brcjhd->bhcrj', q_row, k_win).astype(F32) * scale + bias
        s_ctx = jnp.einsum('bchd,bkhd->bhck', q_row, k_ctx).astype(F32) * scale
        p = jax.nn.softmax(jnp.concatenate([s_ctx, s_loc.reshape(B, H, GRID_W, kr * NA_COLS)], axis=-1), axis=-1)
        p_ctx = p[..., :n_ctx]
        p_loc = p[..., n_ctx:].reshape(B, H, GRID_W, kr, NA_COLS)
        return (jnp.einsum('bhck,bkhd->bchd', p_ctx, v_ctx.astype(F32))
                + jnp.einsum('bhcrj,brcjhd->bchd', p_loc, v_win.astype(F32)))

    q_rows = jnp.moveaxis(q.reshape(B, rows, GRID_W, H, d), 1, 0)
    out = lax.map(row_block, (jnp.arange(rows), q_rows))
    return jnp.moveaxis(out, 0, 1).reshape(B, L, H * d)


def odd_mixer(pc, px, lam, subln, rel_bias, rows, layer_idx, need_ctx):
    B, L, _ = px.shape
    Lc = pc.shape[1]
    dq_c, dk_c, dv_c, nq_c, nk_c, nv_c = split_cols(pc, ODD_WIDTHS)
    dq_x, dk_x, dv_x, nq_x, nk_x, nv_x = split_cols(px, ODD_WIDTHS)
    qk_shape = lambda t: t.reshape(t.shape[:2] + (DIFF_HEADS, 2, DIFF_HEAD))
    v_shape = lambda t: t.reshape(t.shape[:2] + (DIFF_HEADS, DIFF_VHEAD))
    na_shape = lambda t: t.reshape(t.shape[:2] + (NA_HEADS, NA_HEAD))

    lam_init = 0.8 - 0.6 * math.exp(-0.3 * layer_idx)
    lam32 = lam.astype(F32)
    lam_full = jnp.exp(jnp.sum(lam32[0] * lam32[1])) - jnp.exp(jnp.sum(lam32[2] * lam32[3])) + lam_init

    cos, sin = axial_rope_tables(L, DIFF_HEAD)
    cos, sin = cos[:, None, None], sin[:, None, None]
    q_x = apply_axial_rope(qk_shape(dq_x), cos, sin)
    k_all = jnp.concatenate([qk_shape(dk_c), apply_axial_rope(qk_shape(dk_x), cos, sin)], axis=1)
    v_all = jnp.concatenate([v_shape(dv_c), v_shape(dv_x)], axis=1)
    n_blk = L // Q_BLOCK
    q_blocks = jnp.moveaxis(q_x.reshape(B, n_blk, Q_BLOCK, DIFF_HEADS, 2, DIFF_HEAD), 1, 0)
    o = lax.map(lambda qb: diff_attend(qb, k_all, v_all, lam_full), q_blocks)
    o = jnp.moveaxis(o, 0, 1).reshape(B, L, DIFF_HEADS, DIFF_VHEAD)

    def diff_out(t):
        return (rmsnorm(t, subln, SUBLN_EPS) * (1.0 - lam_init)).reshape(t.shape[:2] + (DIFF_WIDTH,))

    na_x = na_latent(na_shape(nq_x), na_shape(nk_x), na_shape(nv_x), na_shape(nk_c), na_shape(nv_c), rel_bias, rows)
    h_x = jnp.concatenate([diff_out(o), na_x], axis=-1).astype(px.dtype)
    h_c = None
    if need_ctx:
        o_c = diff_attend(qk_shape(dq_c), qk_shape(dk_c), v_shape(dv_c), lam_full)
        na_c = dense_attend(na_shape(nq_c), na_shape(nk_c), na_shape(nv_c))
        h_c = jnp.concatenate([diff_out(o_c), na_c.reshape(B, Lc, NA_WIDTH)], axis=-1).astype(pc.dtype)
    return h_x, h_c


def hier_moe(h, wg, bg, we, be, w1, w3, w2):
    T, D = h.shape
    pg = jax.nn.softmax((h @ wg + bg).astype(F32), axis=-1)
    pg_top, g_sel = lax.top_k(pg, 1)
    le = (h @ we + be).astype(F32).reshape(T, N_GROUPS, EXPERTS_PER_GROUP)
    le_g = jnp.take_along_axis(le, g_sel[:, :, None], axis=1)[:, 0]
    pe_top, e_sel = lax.top_k(jax.nn.softmax(le_g, axis=-1), TOP_K_IN_GROUP)
    wts = pg_top * pe_top / jnp.sum(pe_top, axis=-1, keepdims=True)
    eid = g_sel * EXPERTS_PER_GROUP + e_sel
    combine = jnp.sum(jax.nn.one_hot(eid, N_EXPERTS, dtype=F32) * wts[..., None], axis=1)
    out = jnp.zeros((T, D), F32)
    for e in range(N_EXPERTS):
        y = (jax.nn.silu(h @ w1[e]) * (h @ w3[e])) @ w2[e]
        out = out + combine[:, e:e + 1] * y
    return out.astype(h.dtype)


def setup_inputs(seed: int = 0) -> dict:
    key = jax.random.key(seed)
    ks = iter(jax.random.split(key, 40))

    def nrm(shape, scale):
        return jax.random.normal(next(ks), shape, F32) * scale

    D = D_MODEL
    C = RWKV_WIDTH
    inv = D ** -0.5
    return {
        'x': nrm((BATCH, SEQ, D), 1.0),
        'c': nrm((BATCH, D), 1.0),
        'ctx': nrm((BATCH, CTX_LEN, D), 1.0),
        'c_ctx': nrm((D,), 1.0),
        'ada_w': nrm((DEPTH, D, 6 * D), 0.5 * inv),
        'ada_b': nrm((DEPTH, 6 * D), 0.02),
        'norm_g': 1.0 + nrm((DEPTH, 2, D), 0.02),
        'final_g': 1.0 + nrm((D,), 0.02),
        'ev_w_in': nrm((N_EVEN, D, EVEN_PROJ), inv),
        'ev_w_out': nrm((N_EVEN, D, D), inv),
        'ev_decay_w0': nrm((N_EVEN, 2, C), 1.0),
        'ev_decay_up': nrm((N_EVEN, 2, DECAY_LORA, C), DECAY_LORA ** -0.5),
        'ev_iclr_a0': nrm((N_EVEN, 2, C), 0.5),
        'ev_iclr_up': nrm((N_EVEN, 2, ICLR_LORA, C), 0.5 * ICLR_LORA ** -0.5),
        'ev_gate_up': nrm((N_EVEN, GATE_LORA, C), GATE_LORA ** -0.5),
        'ev_k_k': 0.85 + nrm((N_EVEN, C), 0.05),
        'ev_k_a': 1.0 + nrm((N_EVEN, C), 0.05),
        'ev_r_k': nrm((N_EVEN, RWKV_HEADS, RWKV_HEAD), 0.1),
        'ev_lnx_w': 1.0 + nrm((N_EVEN, C), 0.02),
        'ev_lnx_b': nrm((N_EVEN, C), 0.02),
        'ev_conv_w': nrm((N_EVEN, CONV_K, CONV_WIDTH), CONV_K ** -0.5),
        'od_w_in': nrm((N_ODD, D, ODD_PROJ), inv),
        'od_w_out': nrm((N_ODD, D, D), inv),
        'od_lambda': nrm((N_ODD, 4, DIFF_HEAD), 0.1),
        'od_subln': 1.0 + nrm((N_ODD, DIFF_VHEAD), 0.02),
        'od_rel_bias': nrm((N_ODD, NA_HEADS, 2 * NA_MAX_ROWS - 1, 2 * NA_COLS - 1), 0.2),
        'moe_wg': nrm((DEPTH, D, N_GROUPS), inv),
        'moe_bg': nrm((DEPTH, N_GROUPS), 0.01),
        'moe_we': nrm((DEPTH, D, N_EXPERTS), inv),
        'moe_be': nrm((DEPTH, N_EXPERTS), 0.01),
        'moe_w1': nrm((DEPTH, N_EXPERTS, D, D_EXPERT), inv),
        'moe_w3': nrm((DEPTH, N_EXPERTS, D, D_EXPERT), inv),
        'moe_w2': nrm((DEPTH, N_EXPERTS, D_EXPERT, D), D_EXPERT ** -0.5),
    }


def reference(x, c, ctx, c_ctx, ada_w, ada_b, norm_g, final_g,
              ev_w_in, ev_w_out, ev_decay_w0, ev_decay_up, ev_iclr_a0, ev_iclr_up, ev_gate_up,
              ev_k_k, ev_k_a, ev_r_k, ev_lnx_w, ev_lnx_b, ev_conv_w,
              od_w_in, od_w_out, od_lambda, od_subln, od_rel_bias,
              moe_wg, moe_bg, moe_we, moe_be, moe_w1, moe_w3, moe_w2):
    B, L, D = x.shape
    Lc = ctx.shape[1]
    rows = L // GRID_W
    xl, xc = x, ctx
    for l in range(DEPTH):
        need_ctx = l < DEPTH - 1
        i = l // 2
        sh1_x, sc1_x, g1_x, sh2_x, sc2_x, g2_x = [m[:, None, :] for m in ada_mod(c, ada_w[l], ada_b[l])]
        sh1_c, sc1_c, g1_c, sh2_c, sc2_c, g2_c = ada_mod(c_ctx, ada_w[l], ada_b[l])
        hx = modulate(xl, norm_g[l, 0], sh1_x, sc1_x)
        hc = modulate(xc, norm_g[l, 0], sh1_c, sc1_c)
        if l % 2 == 0:
            ox, oc = even_mixer(hc @ ev_w_in[i], hx @ ev_w_in[i], ev_decay_w0[i], ev_decay_up[i],
                                ev_iclr_a0[i], ev_iclr_up[i], ev_gate_up[i], ev_k_k[i], ev_k_a[i],
                                ev_r_k[i], ev_lnx_w[i], ev_lnx_b[i], ev_conv_w[i], need_ctx)
            w_out = ev_w_out[i]
        else:
            ox, oc = odd_mixer(hc @ od_w_in[i], hx @ od_w_in[i], od_lambda[i], od_subln[i],
                               od_rel_bias[i], rows, l, need_ctx)
            w_out = od_w_out[i]
        xl = xl + g1_x * (ox @ w_out)
        hx = modulate(xl, norm_g[l, 1], sh2_x, sc2_x).reshape(B * L, D)
        if need_ctx:
            xc = xc + g1_c * (oc @ w_out)
            hc = modulate(xc, norm_g[l, 1], sh2_c, sc2_c).reshape(B * Lc, D)
            y = hier_moe(jnp.concatenate([hx, hc], axis=0), moe_wg[l], moe_bg[l], moe_we[l], moe_be[l],
                         moe_w1[l], moe_w3[l], moe_w2[l])
            xc = xc + g2_c * y[B * L:].reshape(B, Lc, D)
            y_x = y[:B * L]
        else:
            y_x = hier_moe(hx, moe_wg[l], moe_bg[l], moe_we[l], moe_be[l], moe_w1[l], moe_w3[l], moe_w2[l])
        xl = xl + g2_x * y_x.reshape(B, L, D)
    return rmsnorm(xl, final_g)
```

```python
import contextlib
import math
import numpy as np
import concourse.bass as bass
import concourse.mybir as mybir
from concourse.bass_utils import run_bass_kernel_spmd

F32 = mybir.dt.float32
BF16 = mybir.dt.bfloat16
I32 = mybir.dt.int32
U32 = mybir.dt.uint32
AF = mybir.ActivationFunctionType
ALU = mybir.AluOpType
AX = mybir.AxisListType


class Buf:
    __slots__ = ("name", "writer", "readers")

    def __init__(self, name):
        self.name = name
        self.writer = None
        self.readers = []


class V:
    __slots__ = ("ap", "bufs")

    def __init__(self, ap, bufs):
        self.ap = ap
        self.bufs = tuple(bufs)

    def __getitem__(self, key):
        return V(self.ap[key], self.bufs)

    def re(self, pattern_, **kw):
        return V(self.ap.rearrange(pattern_, **kw), self.bufs)

    def bc(self, shape):
        return V(self.ap.to_broadcast(list(shape)), self.bufs)

    def with_ap(self, ap):
        return V(ap, self.bufs)


class Eng:
    def __init__(self, name, h, sem):
        self.name = name
        self.h = h
        self.sem = sem
        self.count = 0
        self.known = {}
        self.hist = []


class K:
    def __init__(self):
        self.nc = bass.Bass("TRN2", target_bir_lowering=False)
        self.es = contextlib.ExitStack()
        self.root = contextlib.ExitStack()
        self.uid = 0
        nc = self.nc
        self.engs = {}
        for name, h in (("pe", nc.tensor), ("act", nc.scalar), ("dve", nc.vector),
                        ("pool", nc.gpsimd), ("sp", nc.sync)):
            sem = self.root.enter_context(nc.semaphore("s_" + name))
            self.engs[name] = Eng(name, h, sem)
        self.dma_sems = {}
        self.nbuf = 0
        self.same_eng_sync = True
        self.n_inst = 0
        self.n_wait = 0

    def sb(self, name, shape, dtype, nsplit=None):
        self.uid += 1
        t = self.es.enter_context(self.nc.sbuf_tensor("%s_%d" % (name, self.uid), list(shape), dtype))
        return V(t[:], [Buf(name)])

    def ps(self, name, shape, dtype=F32):
        self.uid += 1
        t = self.es.enter_context(self.nc.psum_tensor("%s_%d" % (name, self.uid), list(shape), dtype))
        return V(t[:], [Buf(name)])

    def dram(self, name, shape, dtype, kind="Internal"):
        t = self.nc.dram_tensor(name, list(shape), dtype, kind=kind)
        return V(t.ap(), [Buf(name)])

    def sub(self, v, name):
        return V(v.ap, [Buf(name)])

    def _need(self, eng, dep):
        if dep is None:
            return
        if dep[0] == 'e':
            _, en, idx = dep
            if en == eng.name and (en == "pe" or not self.same_eng_sync):
                return
            key = en
            val = idx
        else:
            _, key, val = dep
        if eng.known.get(key, 0) >= val:
            return
        if dep[0] == 'e':
            sem = self.engs[en].sem
        else:
            sem = self.dma_sems[key][0]
        eng.h.wait_ge(sem, val)
        self.n_wait += 1
        nk = dict(eng.known)
        nk[key] = val
        if dep[0] == 'e' and en != eng.name:
            other = self.engs[en].hist[idx - 1]
            for k2, v2 in other.items():
                if nk.get(k2, 0) < v2:
                    nk[k2] = v2
        eng.known = nk

    def emit(self, engname, fn, reads=(), writes=()):
        eng = self.engs[engname]
        for v in reads:
            for b in v.bufs:
                self._need(eng, b.writer)
        for v in writes:
            for b in v.bufs:
                self._need(eng, b.writer)
                for r in b.readers:
                    self._need(eng, r)
        ins = fn(eng.h)
        eng.count += 1
        ins.then_inc(eng.sem, 1)
        me = ('e', engname, eng.count)
        eng.hist.append(eng.known)
        for v in reads:
            for b in v.bufs:
                b.readers.append(me)
        for v in writes:
            for b in v.bufs:
                b.writer = me
                b.readers = []
        self.n_inst += 1
        return ins

    def dma(self, qname, out, in_, **kw):
        eng = self.engs[qname]
        for b in in_.bufs:
            self._need(eng, b.writer)
        for b in out.bufs:
            self._need(eng, b.writer)
            for r in b.readers:
                self._need(eng, r)
        key = out.bufs[0].name
        if key not in self.dma_sems:
            sem = self.root.enter_context(self.nc.semaphore("d_" + key))
            self.dma_sems[key] = [sem, 0]
        ent = self.dma_sems[key]
        ent[1] += 16
        ins = eng.h.dma_start(out=out.ap, in_=in_.ap, **kw)
        ins.then_inc(ent[0], 16)
        me = ('d', key, ent[1])
        for b in in_.bufs:
            b.readers.append(me)
        for b in out.bufs:
            b.writer = me
            b.readers = []
        self.n_inst += 1
        return ins

    def wait_all(self, engname, vs):
        eng = self.engs[engname]
        for v in vs:
            for b in v.bufs:
                self._need(eng, b.writer)

    def mm(self, out, lhsT, rhs, start=True, stop=True):
        rd = [lhsT, rhs]
        return self.emit("pe", lambda h: h.matmul(out.ap, lhsT=lhsT.ap, rhs=rhs.ap, start=start, stop=stop),
                         reads=rd, writes=[out])

    def tr(self, out, in_, ident):
        return self.emit("pe", lambda h: h.transpose(out.ap, in_.ap, ident.ap), reads=[in_, ident], writes=[out])

    def act(self, out, in_, func, bias=None, scale=None, accum_out=None):
        rd = [in_]
        kw = {}
        if bias is not None:
            if isinstance(bias, V):
                rd.append(bias); kw["bias"] = bias.ap
            else:
                kw["bias"] = bias
        if scale is not None:
            if isinstance(scale, V):
                rd.append(scale); kw["scale"] = scale.ap
            else:
                kw["scale"] = scale
        wr = [out]
        if accum_out is not None:
            wr.append(accum_out); kw["accum_out"] = accum_out.ap
        return self.emit("act", lambda h: h.activation(out=out.ap, in_=in_.ap, func=func, **kw), reads=rd, writes=wr)

    def tt(self, e, out, in0, in1, op):
        return self.emit(e, lambda h: h.tensor_tensor(out=out.ap, in0=in0.ap, in1=in1.ap, op=op),
                         reads=[in0, in1], writes=[out])

    def ts(self, e, out, in0, s1, op0, s2=None, op1=None, accum_out=None):
        rd = [in0]
        a1 = s1
        if isinstance(s1, V):
            rd.append(s1); a1 = s1.ap
        a2 = s2
        if isinstance(s2, V):
            rd.append(s2); a2 = s2.ap
        kw = {}
        if op1 is not None:
            kw["op1"] = op1
        wr = [out]
        if accum_out is not None:
            wr.append(accum_out); kw["accum_out"] = accum_out.ap
        return self.emit(e, lambda h: h.tensor_scalar(out=out.ap, in0=in0.ap, scalar1=a1, scalar2=a2, op0=op0, **kw),
                         reads=rd, writes=wr)

    def stt(self, e, out, in0, scalar, in1, op0, op1):
        rd = [in0, in1]
        a = scalar
        if isinstance(scalar, V):
            rd.append(scalar); a = scalar.ap
        return self.emit(e, lambda h: h.scalar_tensor_tensor(out=out.ap, in0=in0.ap, scalar=a, in1=in1.ap, op0=op0, op1=op1),
                         reads=rd, writes=[out])

    def copy(self, e, out, in_):
        if e == "act":
            return self.emit(e, lambda h: h.copy(out=out.ap, in_=in_.ap), reads=[in_], writes=[out])
        return self.emit(e, lambda h: h.tensor_copy(out=out.ap, in_=in_.ap), reads=[in_], writes=[out])

    def memset(self, e, out, val):
        return self.emit(e, lambda h: h.memset(out.ap, val), reads=[], writes=[out])

    def reduce(self, e, out, in_, op, axis=AX.X):
        return self.emit(e, lambda h: h.tensor_reduce(out=out.ap, in_=in_.ap, axis=axis, op=op), reads=[in_], writes=[out])

    def recip(self, out, in_):
        return self.emit("dve", lambda h: h.reciprocal(out=out.ap, in_=in_.ap), reads=[in_], writes=[out])

    def barrier(self):
        for en, e in self.engs.items():
            for en2, e2 in self.engs.items():
                if e2.count > 0 and (en2 != en or en != "pe"):
                    self._need(e, ('e', en2, e2.count))
            for key, (sem, cnt) in self.dma_sems.items():
                self._need(e, ('d', key, cnt))

    @contextlib.contextmanager
    def scope(self):
        old = self.es
        self.es = contextlib.ExitStack()
        self._scope_depth = getattr(self, "_scope_depth", 0) + 1
        try:
            yield
        finally:
            self.barrier()
            self.es.close()
            self.es = old
            self._scope_depth -= 1

    def rsqrt(self, out, in_, mul, add):
        self.ts("dve", out, in_, mul, ALU.mult, add, ALU.add)
        self.emit("act", lambda h: h.sqrt(out=out.ap, in_=out.ap), reads=[out], writes=[out])
        self.recip(out, out)

    def rsqrt_from(self, out, in_, mul, add):
        self.ts("dve", out, in_, mul, ALU.mult, add, ALU.add)
        self.emit("act", lambda h: h.sqrt(out=out.ap, in_=out.ap), reads=[out], writes=[out])
        self.recip(out, out)

    def finish(self, outs):
        sp = self.engs["sp"]
        for v in outs:
            for b in v.bufs:
                self._need(sp, b.writer)
        for en, e in self.engs.items():
            if en != "sp" and e.count > 0:
                self._need(sp, ('e', en, e.count))
        for key, (sem, cnt) in self.dma_sems.items():
            self._need(sp, ('d', key, cnt))


NT = 18
TOK = 2304
D = 1024
RMS_EPS_ = 1e-6

WSPECS = [
    ("ada_w", [4, 1024, 6144]), ("ada_b", [4, 6144]), ("norm_g", [4, 2, 1024]), ("final_g", [1, 1024]),
    ("ev_w_in", [2, 1024, 3456]), ("ev_w_out", [2, 1024, 1024]), ("ev_decay_w0", [2, 2, 512]),
    ("ev_decay_up", [2, 2, 64, 512]), ("ev_iclr_a0", [2, 2, 512]), ("ev_iclr_up", [2, 2, 64, 512]),
    ("ev_gate_up", [2, 128, 512]), ("ev_k_k", [2, 512]), ("ev_k_a", [2, 512]), ("ev_r_k", [2, 512]),
    ("ev_lnx_w", [2, 512]), ("ev_lnx_b", [2, 512]), ("ev_conv_w", [2, 3, 512]),
    ("od_w_in", [2, 1024, 3072]), ("od_w_out", [2, 1024, 1024]), ("od_lambda", [2, 256]),
    ("od_subln", [2, 128]), ("od_rel_bias", [2, 8, 15, 31]),
    ("moe_wg", [4, 1024, 4]), ("moe_bg", [4, 4]), ("moe_we", [4, 1024, 16]), ("moe_be", [4, 16]),
    ("moe_w1", [4, 16, 1024, 512]), ("moe_w3", [4, 16, 1024, 512]), ("moe_w2", [4, 16, 512, 1024]),
]


class G:
    pass


def build_program(layers=(0, 1, 2, 3), do_mixer=True, final=True):
    k = K()
    g = G()
    g.k = k
    W = {}
    xin = k.dram("xin", [TOK, D], F32, "ExternalInput")
    cc = k.dram("cc", [2, D], F32, "ExternalInput")
    for name, shape in WSPECS:
        W[name] = k.dram(name, shape, F32, "ExternalInput")
    out = k.dram("out", [2048, D], F32, "ExternalOutput")
    g.W = W
    nam, g.na_cls = na_tables()
    g.n_na_cls = nam.shape[0]
    g.namask = k.dram("namask", list(nam.shape), F32, "ExternalInput")
    g.nabias = k.dram("nabias", [2, 8, 128, 16, 64], F32, "ExternalInput")
    g.ropec = k.dram("ropec", [128, 2048], F32, "ExternalInput")
    g.ropes = k.dram("ropes", [128, 2048], F32, "ExternalInput")
    g.tri = k.dram("tri", [6, 64, 64], F32, "ExternalInput")
    g.trif = k.dram("trif", [2, 64, 128], F32, "ExternalInput")

    xs = k.sb("xs", [128, NT, D], F32)
    g.xs_t = [V(xs.ap[:, tt, :], [Buf("xs%d" % tt)]) for tt in range(NT)]
    hT = k.sb("hT", [128, 8, TOK], BF16)
    g.hT = hT
    g.hT_bufs = [Buf("hT%d" % tt) for tt in range(NT)]

    def hTv(kc, t0, t1):
        return V(hT.ap[:, kc, t0:t1], g.hT_bufs[t0 // 128:(t1 + 127) // 128])
    g.hTv = hTv

    identf = k.sb("identf", [128, 128], F32)
    ident = k.sb("ident", [128, 128], BF16)
    k.memset("dve", identf, 0.0)
    k.emit("pool", lambda h: h.affine_select(out=identf.ap, in_=identf.ap, pattern=[[-1, 128]],
                                            compare_op=ALU.not_equal, fill=1.0, base=0, channel_multiplier=1),
           reads=[identf], writes=[identf])
    k.copy("dve", ident, identf)
    g.ident = ident
    g.identf = identf

    for tt in range(NT):
        k.dma("sp", g.xs_t[tt], V(xin.ap[tt * 128:(tt + 1) * 128, :], xin.bufs))

    siluL = k.sb("siluL", [128, 2, 8, 128], BF16)
    with k.scope():
        ccT = k.sb("ccT", [128, 2, 8], F32)
        ccS = k.sb("ccS", [128, 2, 8], F32)
        k.dma("sp", ccT, V(cc.ap.rearrange("r (kc p) -> p r kc", p=128), cc.bufs), allow_slow_non_contiguous=True)
        k.act(ccS, ccT, AF.Silu)
        for r in range(2):
            for kc in range(8):
                k.copy("dve", siluL[:, r, kc, :], ccS[:, r, kc:kc + 1].bc([128, 128]))
    g.siluL = siluL

    for l in layers:
        need_ctx = l < 3
        with k.scope():
            g1t = k.sb("g1t", [128, 2, 1024], F32)
            with k.scope():
                mod = k.sb("mod", [128, 2, 3072], F32)
                ada_stage(g, l, 0, mod)
                norm_phase(g, l, 0, mod, NT)
                k.copy("pool", g1t, mod[:, :, 2048:3072])
            g.g1t = g1t
            if do_mixer:
                if l % 2 == 0:
                    even_mixer(g, l, need_ctx)
                else:
                    odd_mixer(g, l, need_ctx)
        with k.scope():
            mod = k.sb("mod", [128, 2, 3072], F32)
            ada_stage(g, l, 1, mod)
            ntl = NT if need_ctx else 16
            norm_phase(g, l, 1, mod, ntl)
            moe_phase(g, l, mod, ntl)

    with k.scope():
        fg = k.sb("fg", [128, D], F32)
        k.dma("sp", fg, V(W["final_g"].ap[0:1, :].partition_broadcast(128), W["final_g"].bufs))
        junk = k.sb("junk", [128, D], BF16)
        ss = k.sb("ss", [128, NT], F32)
        rstd = k.sb("rstd", [128, NT], F32)
        k.memset("dve", ss, 0.0)
        ot = [k.sb("ot%d" % i, [128, D], F32) for i in range(2)]
        for tt in range(16):
            o = ot[tt % 2]
            if final:
                k.act(junk, g.xs_t[tt], AF.Square, accum_out=ss[:, tt:tt + 1])
                k.rsqrt(rstd[:, tt:tt + 1], ss[:, tt:tt + 1], 1.0 / D, RMS_EPS_)
                k.stt("dve", o, g.xs_t[tt], rstd[:, tt:tt + 1], fg, ALU.mult, ALU.mult)
            else:
                k.copy("dve", o, g.xs_t[tt])
            k.dma("sp", V(out.ap[tt * 128:(tt + 1) * 128, :], out.bufs), o)
    k.finish([out])
    return k


def ada_stage(g, l, st, mod):
    k = g.k
    W = g.W
    with k.scope():
        wch = [k.sb("adaw%d" % i, [128, 8, 512], BF16) for i in range(2)]
        bch = [k.sb("adab%d" % i, [128, 512], F32) for i in range(2)]
        pA = [k.ps("adap%d" % i, [128, 512]) for i in range(2)]
        for ci in range(6):
            c0 = st * 3072 + ci * 512
            w = wch[ci % 2]
            b = bch[ci % 2]
            k.dma("pool", w, V(W["ada_w"].ap[l, :, c0:c0 + 512].rearrange("(kc p) c -> p kc c", p=128), W["ada_w"].bufs))
            k.dma("sp", b, V(W["ada_b"].ap[l:l + 1, c0:c0 + 512].partition_broadcast(128), W["ada_b"].bufs))
            for r in range(2):
                p = pA[r]
                for kc in range(8):
                    k.mm(p, g.siluL[:, r, kc, :], w[:, kc, :], start=(kc == 0), stop=(kc == 7))
                k.tt("dve", mod[:, r, ci * 512:(ci + 1) * 512], p, b, ALU.add)


def norm_phase(g, l, st, mod, ntiles):
    k = g.k
    W = g.W
    with k.scope():
        ng = k.sb("ng", [128, D], F32)
        k.dma("sp", ng, V(W["norm_g"].ap[l, st:st + 1, :].partition_broadcast(128), W["norm_g"].bufs))
        for r in range(2):
            k.stt("dve", mod[:, r, 1024:2048], mod[:, r, 1024:2048], 1.0, ng, ALU.add, ALU.mult)
        junk = k.sb("junk", [128, D], BF16)
        ss = k.sb("ss", [128, NT], F32)
        rstd = k.sb("rstd", [128, NT], F32)
        k.memset("dve", ss, 0.0)
        tmp = [k.sb("ntmp%d" % i, [128, D], F32) for i in range(2)]
        hb = [k.sb("hb%d" % i, [128, D], BF16) for i in range(2)]
        pT = [k.ps("pT%d" % i, [128, 8, 128], BF16) for i in range(2)]
        for tt in range(ntiles):
            i = tt % 2
            r = 0 if tt < 16 else 1
            k.act(junk, g.xs_t[tt], AF.Square, accum_out=ss[:, tt:tt + 1])
            k.rsqrt(rstd[:, tt:tt + 1], ss[:, tt:tt + 1], 1.0 / D, RMS_EPS_)
            k.stt("dve", tmp[i], g.xs_t[tt], rstd[:, tt:tt + 1], mod[:, r, 1024:2048], ALU.mult, ALU.mult)
            k.tt("pool", hb[i], tmp[i], mod[:, r, 0:1024], ALU.add)
            for kc in range(8):
                k.tr(pT[i][:, kc, :], hb[i][:, kc * 128:(kc + 1) * 128], g.ident)
            k.copy("act", V(g.hT.ap[:, :, tt * 128:(tt + 1) * 128], [g.hT_bufs[tt]]), pT[i])


def moe_phase(g, l, mod, ntiles):
    k = g.k
    W = g.W
    with k.scope():
        wr = k.sb("wr", [128, 8, 20], BF16)
        k.dma("pool", wr[:, :, 0:4], V(W["moe_wg"].ap[l].rearrange("(kc p) c -> p kc c", p=128), W["moe_wg"].bufs),
              allow_slow_non_contiguous=True)
        k.dma("pool", wr[:, :, 4:20], V(W["moe_we"].ap[l].rearrange("(kc p) c -> p kc c", p=128), W["moe_we"].bufs),
              allow_slow_non_contiguous=True)
        rb = k.sb("rb", [128, 20], F32)
        k.dma("sp", rb[:, 0:4], V(W["moe_bg"].ap[l:l + 1, :].partition_broadcast(128), W["moe_bg"].bufs))
        k.dma("sp", rb[:, 4:20], V(W["moe_be"].ap[l:l + 1, :].partition_broadcast(128), W["moe_be"].bufs))
        comb = k.sb("comb", [128, NT, 16], F32)
        with k.scope():
            pr = [k.ps("pr%d" % i, [128, 20]) for i in range(2)]
            lg = k.sb("lg", [128, 20], F32)
            sm = k.sb("rsm", [128, 16], F32)
            oh = k.sb("oh", [128, 4], F32)
            eg = k.sb("eg", [128, 4], F32)
            t44 = k.sb("t44", [128, 4, 4], F32)
            leg8 = k.sb("leg8", [128, 8], F32)
            m8 = k.sb("m8", [128, 8], F32)
            selm = k.sb("selm", [128, 4], F32)
            ee = k.sb("ee", [128, 4], F32)
            wts = k.sb("wts", [128, 4], F32)
            k.memset("dve", leg8, -1e30)
            k.memset("dve", sm, 0.0)
            for tt in range(ntiles):
                p = pr[tt % 2]
                for kc in range(8):
                    k.mm(p, g.hTv(kc, tt * 128, (tt + 1) * 128), wr[:, kc, :], start=(kc == 0), stop=(kc == 7))
                k.tt("dve", lg, p, rb, ALU.add)
                gmax, ngmax, sg, nm1, s2, den = (sm[:, j:j + 1] for j in range(6))
                k.reduce("dve", gmax, lg[:, 0:4], ALU.max)
                k.ts("dve", oh, lg[:, 0:4], gmax, ALU.is_equal)
                k.ts("dve", ngmax, gmax, -1.0, ALU.mult)
                k.act(eg, lg[:, 0:4], AF.Exp, bias=ngmax)
                k.reduce("dve", sg, eg, ALU.add)
                k.tt("dve", t44, lg[:, 4:20].re("p (g e) -> p g e", g=4),
                     oh.re("p (g o) -> p g o", o=1).bc([128, 4, 4]), ALU.mult)
                k.reduce("dve", leg8[:, 0:4], t44.re("p g e -> p e g"), ALU.add)
                k.emit("dve", lambda h: h.max(out=m8.ap, in_=leg8.ap), reads=[leg8], writes=[m8])
                k.ts("dve", selm, leg8[:, 0:4], m8[:, 1:2], ALU.is_ge)
                k.ts("dve", nm1, m8[:, 0:1], -1.0, ALU.mult)
                k.act(ee, leg8[:, 0:4], AF.Exp, bias=nm1)
                k.tt("dve", ee, ee, selm, ALU.mult)
                k.reduce("dve", s2, ee, ALU.add)
                k.tt("dve", den, s2, sg, ALU.mult)
                k.recip(den, den)
                k.ts("dve", wts, ee, den, ALU.mult)
                k.tt("dve", comb[:, tt, :].re("p (g e) -> p g e", g=4),
                     oh.re("p (g o) -> p g o", o=1).bc([128, 4, 4]),
                     wts.re("p (o e) -> p o e", o=1).bc([128, 4, 4]), ALU.mult)
        with k.scope():
            w1b = [k.sb("w1b%d" % i, [128, 8, 512], BF16) for i in range(2)]
            w3b = [k.sb("w3b%d" % i, [128, 8, 512], BF16) for i in range(2)]
            w2b = [k.sb("w2b%d" % i, [128, 4, 1024], BF16) for i in range(2)]
            pa = [k.ps("pa%d" % i, [128, 512]) for i in range(2)]
            pb = [k.ps("pb%d" % i, [128, 512]) for i in range(2)]
            py = [k.ps("py%d" % i, [128, 512]) for i in range(2)]
            sA = [k.sb("sA%d" % i, [128, 512], BF16) for i in range(2)]
            gt = [[k.sb("gt%d_%d" % (i, fc), [128, 512], BF16) for fc in range(4)] for i in range(2)]
            ytmp = [k.sb("ytmp%d" % i, [128, 512], F32) for i in range(2)]
            blocks = [(b * 512, 512) for b in range(4)]
            if ntiles == NT:
                blocks.append((2048, 256))
            cnt = 0
            for e in range(16):
                s = e % 2
                k.dma("pool", w1b[s], V(W["moe_w1"].ap[l, e].rearrange("(kc p) f -> p kc f", p=128), W["moe_w1"].bufs))
                k.dma("pool", w3b[s], V(W["moe_w3"].ap[l, e].rearrange("(kc p) f -> p kc f", p=128), W["moe_w3"].bufs))
                k.dma("pool", w2b[s], V(W["moe_w2"].ap[l, e].rearrange("(fc p) d -> p fc d", p=128), W["moe_w2"].bufs))
                for bi, (t0, n) in enumerate(blocks):
                    gi = bi % 2
                    for fc in range(4):
                        a = pa[fc % 2]
                        b = pb[fc % 2]
                        for kc in range(8):
                            k.mm(a[:, 0:n], w1b[s][:, kc, fc * 128:(fc + 1) * 128], g.hTv(kc, t0, t0 + n),
                                 start=(kc == 0), stop=(kc == 7))
                        for kc in range(8):
                            k.mm(b[:, 0:n], w3b[s][:, kc, fc * 128:(fc + 1) * 128], g.hTv(kc, t0, t0 + n),
                                 start=(kc == 0), stop=(kc == 7))
                        k.act(sA[fc % 2][:, 0:n], a[:, 0:n], AF.Silu)
                        k.tt("dve", gt[gi][fc][:, 0:n], sA[fc % 2][:, 0:n], b[:, 0:n], ALU.mult)
                    for ti in range(n // 128):
                        tt = t0 // 128 + ti
                        r = 0 if tt < 16 else 1
                        for dh in range(2):
                            y = py[cnt % 2]
                            yt = ytmp[cnt % 2]
                            cnt += 1
                            for fc in range(4):
                                k.mm(y, gt[gi][fc][:, ti * 128:(ti + 1) * 128], w2b[s][:, fc, dh * 512:(dh + 1) * 512],
                                     start=(fc == 0), stop=(fc == 3))
                            k.stt("dve", yt, y, comb[:, tt, e:e + 1], mod[:, r, 2048 + dh * 512:2048 + (dh + 1) * 512],
                                  ALU.mult, ALU.mult)
                            xv = g.xs_t[tt][:, dh * 512:(dh + 1) * 512]
                            k.tt("pool", xv, xv, yt, ALU.add)


def out_proj_chunk(g, wsrc, li, row0, oTc, ntiles, tagi):
    k = g.k
    wo = g.wo_t[tagi % 2]
    k.dma("pool", wo, V(wsrc.ap[li, row0:row0 + 128, :], wsrc.bufs))
    for tt in range(ntiles):
        r = 0 if tt < 16 else 1
        for dh in range(2):
            y = g.po_t[(tt * 2 + dh) % 2]
            yt = g.otmp_t[(tt * 2 + dh) % 2]
            k.mm(y, oTc[:, tt * 128:(tt + 1) * 128], wo[:, dh * 512:(dh + 1) * 512])
            k.tt("dve", yt, y, g.g1t[:, r, dh * 512:(dh + 1) * 512], ALU.mult)
            xv = g.xs_t[tt][:, dh * 512:(dh + 1) * 512]
            k.tt("pool", xv, xv, yt, ALU.add)


def na_tables():
    rows = 32
    def rs(rq):
        return min(max(rq - 4, 0), rows - 8)
    def cs(cq):
        return min(max(cq - 8, 0), 64 - 16)
    colm = np.zeros((64, 64), np.float32)
    for cq in range(64):
        colm[cs(cq):cs(cq) + 16, cq] = 1.0
    classes = []
    keys = {}
    cls = {}
    for kt in range(16):
        for j in range(max(0, kt - 3), min(15, kt + 3) + 1):
            pat = []
            for rkl in range(2):
                for rql in range(2):
                    rk = 2 * kt + rkl
                    rq = 2 * j + rql
                    pat.append(1 if rs(rq) <= rk < rs(rq) + 8 else 0)
            pat = tuple(pat)
            if sum(pat) == 0:
                continue
            if pat not in keys:
                m = np.zeros((128, 128), np.float32)
                for rkl in range(2):
                    for rql in range(2):
                        if pat[rkl * 2 + rql]:
                            m[rkl * 64:(rkl + 1) * 64, rql * 64:(rql + 1) * 64] = colm
                keys[pat] = len(classes)
                classes.append(m)
            cls[(kt, j)] = keys[pat]
    return np.stack(classes, 0), cls


def rope_tables():
    inv = 10000.0 ** (-np.arange(16, dtype=np.float64) / 16.0)
    t = np.arange(2048)
    pos = np.stack([t // 64, t % 64], 0).astype(np.float64)
    cosT = np.zeros((128, 2048), np.float32)
    sinT = np.zeros((128, 2048), np.float32)
    for p in range(128):
        d = p % 64
        ax = 0 if d < 32 else 1
        f = d % 16
        ang = (pos[ax].astype(np.float32) * np.float32(inv[f])).astype(np.float32)
        cosT[p] = np.cos(ang)
        sinT[p] = np.sin(ang)
    return cosT, sinT


def na_bias_layout(rb):
    rkl = np.arange(2)[:, None, None, None]
    ck = np.arange(64)[None, :, None, None]
    u = np.arange(16)[None, None, :, None]
    cq = np.arange(64)[None, None, None, :]
    dr = rkl + 14 - u + 0 * ck + 0 * cq
    dc = ck - cq + 15 + 0 * rkl + 0 * u
    ok = (dr >= 0) & (dr <= 14) & (dc >= 0) & (dc <= 30)
    drc = np.clip(dr, 0, 14)
    dcc = np.clip(dc, 0, 30)
    t = rb[:, :, drc, dcc]
    t = np.where(ok[None, None], t, np.float32(0.0))
    return np.ascontiguousarray(t.reshape(2, 8, 128, 16, 64).astype(np.float32))


def odd_mixer(g, l, need_ctx):
    k = g.k
    W = g.W
    i = l // 2
    lam_init = 0.8 - 0.6 * math.exp(-0.3 * l)
    ntl = NT if need_ctx else 16
    win = W["od_w_in"]
    hTv = g.hTv
    with k.scope():
        g.wo_t = [k.sb("wo%d" % j, [128, 1024], BF16) for j in range(2)]
        g.otmp_t = [k.sb("otmp%d" % j, [128, 512], F32) for j in range(2)]
        onesb = k.sb("onesb", [128, 128], BF16)
        k.memset("dve", onesb, 1.0)
        oTc = [k.sb("oTc%d" % j, [128, TOK], BF16) for j in range(2)]
        QT = k.sb("QT", [128, TOK], BF16)
        KT = k.sb("KT", [128, TOK], BF16)
        VV = k.sb("VV", [128, NT, 128], BF16)
        wq = k.sb("wq", [128, 8, 128], BF16)
        wk = k.sb("wk", [128, 8, 128], BF16)
        wv = k.sb("wv", [128, 8, 128], BF16)
        lam = k.sb("lam", [128, 256], F32)
        k.dma("sp", lam, V(W["od_lambda"].ap[i:i + 1, :].partition_broadcast(128), W["od_lambda"].bufs))
        lsm = k.sb("lsm", [128, 8], F32)
        ltmp = k.sb("ltmp", [128, 64], F32)
        k.tt("dve", ltmp, lam[:, 0:64], lam[:, 64:128], ALU.mult)
        k.reduce("dve", lsm[:, 0:1], ltmp, ALU.add)
        k.tt("dve", ltmp, lam[:, 128:192], lam[:, 192:256], ALU.mult)
        k.reduce("dve", lsm[:, 1:2], ltmp, ALU.add)
        k.act(lsm[:, 2:4], lsm[:, 0:2], AF.Exp)
        k.tt("dve", lsm[:, 4:5], lsm[:, 3:4], lsm[:, 2:3], ALU.subtract)
        k.ts("dve", lsm[:, 5:6], lsm[:, 4:5], -lam_init, ALU.add)
        nlam = lsm[:, 5:6]
        sub = k.sb("subln", [128, 1], F32)
        k.dma("sp", sub, V(W["od_subln"].ap[i:i + 1, :].rearrange("o p -> p o"), W["od_subln"].bufs),
              allow_slow_non_contiguous=True)
        k.ts("dve", sub, sub, 1.0 - lam_init, ALU.mult)

        with k.scope():
            g.po_t = [k.ps("po%d" % j, [128, 512]) for j in range(2)]
            cosT = k.sb("cosT", [128, 2048], F32)
            sinT = k.sb("sinT", [128, 2048], F32)
            k.dma("sp", cosT, g.ropec)
            k.dma("sp", sinT, g.ropes)
            wq2 = k.sb("wq2", [128, 8, 128], BF16)
            wk2 = k.sb("wk2", [128, 8, 128], BF16)
            pp = [k.ps("pp%d" % j, [128, 512]) for j in range(2)]
            ps_ = [k.ps("pS%d" % j, [128, 512]) for j in range(2)]
            pnum = k.ps("pnum", [128, 512])
            pden = k.ps("pden", [128, 512])
            rt1 = k.sb("rt1", [128, 512], F32)
            rt2 = k.sb("rt2", [128, 512], F32)
            Eb = [k.sb("Eb%d" % j, [128, 512], BF16) for j in range(3)]
            Om = [k.sb("Om%d" % j, [128, 512], F32) for j in range(2)]
            rden = k.sb("rden", [128, 512], F32)
            Of = k.sb("Of", [128, 512], F32)
            sqb = k.sb("sqb", [128, 512], BF16)
            rsn = k.sb("rsn", [128, 512], F32)
            ecnt = 0
            for h in range(4):
                oc = oTc[h % 2]
                k.dma("pool", wq, V(win.ap[i, :, h * 128:(h + 1) * 128].rearrange("(kc p) c -> p kc c", p=128), win.bufs))
                k.dma("pool", wk, V(win.ap[i, :, 512 + h * 128:512 + (h + 1) * 128].rearrange("(kc p) c -> p kc c", p=128), win.bufs))
                k.dma("pool", wv, V(win.ap[i, :, 1024 + h * 128:1024 + (h + 1) * 128].rearrange("(kc p) c -> p kc c", p=128), win.bufs))
                for (ws, wd) in ((wq, wq2), (wk, wk2)):
                    for kc in range(8):
                        s4 = ws[:, kc, :].re("p (b t s) -> p b t s", b=4, t=2, s=16)
                        d4 = wd[:, kc, :].re("p (b t s) -> p b t s", b=4, t=2, s=16)
                        k.ts("pool", d4[:, :, 0, :], s4[:, :, 1, :], -1.0, ALU.mult)
                        k.copy("pool", d4[:, :, 1, :], s4[:, :, 0, :])
                nblk = 5
                for bi in range(nblk):
                    t0 = bi * 512
                    n = 512 if bi < 4 else 256
                    for (ws, wd, dst) in ((wq, wq2, QT), (wk, wk2, KT)):
                        if dst is QT and bi == 4 and not need_ctx:
                            continue
                        p1 = pp[0]
                        for kc in range(8):
                            k.mm(p1[:, 0:n], ws[:, kc, :], hTv(kc, t0, t0 + n), start=(kc == 0), stop=(kc == 7))
                        if bi < 4:
                            p2 = pp[1]
                            for kc in range(8):
                                k.mm(p2[:, 0:n], wd[:, kc, :], hTv(kc, t0, t0 + n), start=(kc == 0), stop=(kc == 7))
                            k.tt("dve", rt1, p1, cosT[:, t0:t0 + n], ALU.mult)
                            k.tt("dve", rt2, p2, sinT[:, t0:t0 + n], ALU.mult)
                            k.tt("pool", dst[:, t0:t0 + n], rt1, rt2, ALU.add)
                        else:
                            k.copy("act", dst[:, t0:t0 + n], p1[:, 0:n])
                for tt in range(NT):
                    p1 = pp[tt % 2]
                    for kc in range(8):
                        k.mm(p1[:, 0:128], hTv(kc, tt * 128, (tt + 1) * 128), wv[:, kc, :], start=(kc == 0), stop=(kc == 7))
                    k.copy("act", VV[:, tt, :], p1[:, 0:128])
                qblocks = [(b * 512, 512, list(range(NT))) for b in range(4)]
                if need_ctx:
                    qblocks.append((2048, 256, [16, 17]))
                for (q0, n, kts) in qblocks:
                    for m in range(2):
                        for ki, kt in enumerate(kts):
                            S = ps_[ecnt % 2]
                            E = Eb[ecnt % 3]
                            ecnt += 1
                            k.mm(S[:, 0:n], KT[m * 64:(m + 1) * 64, kt * 128:(kt + 1) * 128], QT[m * 64:(m + 1) * 64, q0:q0 + n])
                            k.act(E[:, 0:n], S[:, 0:n], AF.Exp, scale=0.125)
                            k.mm(pnum[:, 0:n], VV[:, kt, :], E[:, 0:n], start=(ki == 0), stop=(ki == len(kts) - 1))
                            k.mm(pden[:, 0:n], onesb, E[:, 0:n], start=(ki == 0), stop=(ki == len(kts) - 1))
                        k.recip(rden[:, 0:n], pden[:, 0:n])
                        k.tt("dve", Om[m][:, 0:n], pnum[:, 0:n], rden[:, 0:n], ALU.mult)
                    k.stt("dve", Of[:, 0:n], Om[1][:, 0:n], nlam, Om[0][:, 0:n], ALU.mult, ALU.add)
                    k.act(sqb[:, 0:n], Of[:, 0:n], AF.Square)
                    k.mm(pden[:, 0:n], onesb, sqb[:, 0:n])
                    k.rsqrt_from(rsn[:, 0:n], pden[:, 0:n], 1.0 / 128, 1e-5)
                    k.stt("dve", oc[:, q0:q0 + n], Of[:, 0:n], sub, rsn[:, 0:n], ALU.mult, ALU.mult)
                out_proj_chunk(g, W["od_w_out"], i, h * 128, oc, ntl, h)

        with k.scope():
            g.po_t = [k.ps("po%d" % j, [128, 512]) for j in range(2)]
            nmask = k.sb("nmask", [128, g.n_na_cls, 128], BF16)
            k.dma("pool", nmask, V(g.namask.ap.rearrange("c p q -> p c q"), g.namask.bufs))
            TB = k.sb("TB", [128, 16, 64], F32)
            EL = k.sb("EL", [128, 16, 896], BF16)
            EC = k.sb("EC", [128, 2, 2048], BF16)
            pp1 = k.ps("pp0", [128, 512])
            pp = [pp1, pp1]
            pS = [k.ps("pS%d" % j, [128, 1024]) for j in range(2)]
            pnd = k.ps("pnd", [128, 256])
            pnum = V(pnd.ap[:, 0:128], [Buf("pnum")])
            pden = V(pnd.ap[:, 128:256], [Buf("pden")])
            stmp = k.sb("stmp", [128, 896], F32)
            rden = k.sb("rden", [128, 128], F32)
            cnt = 0
            for hp in range(4):
                oc = oTc[hp % 2]
                c0 = 1536 + hp * 128
                k.dma("pool", wq, V(win.ap[i, :, c0:c0 + 128].rearrange("(kc p) c -> p kc c", p=128), win.bufs))
                k.dma("pool", wk, V(win.ap[i, :, 512 + c0:512 + c0 + 128].rearrange("(kc p) c -> p kc c", p=128), win.bufs))
                k.dma("pool", wv, V(win.ap[i, :, 1024 + c0:1024 + c0 + 128].rearrange("(kc p) c -> p kc c", p=128), win.bufs))
                for bi in range(5):
                    t0 = bi * 512
                    n = 512 if bi < 4 else 256
                    for (ws, dst) in ((wq, QT), (wk, KT)):
                        if dst is QT and bi == 4 and not need_ctx:
                            continue
                        p1 = pp[cnt % 2]
                        cnt += 1
                        for kc in range(8):
                            k.mm(p1[:, 0:n], ws[:, kc, :], hTv(kc, t0, t0 + n), start=(kc == 0), stop=(kc == 7))
                        k.copy("act", dst[:, t0:t0 + n], p1[:, 0:n])
                for tt in range(NT):
                    p1 = pp[cnt % 2]
                    cnt += 1
                    for kc in range(8):
                        k.mm(p1[:, 0:128], hTv(kc, tt * 128, (tt + 1) * 128), wv[:, kc, :], start=(kc == 0), stop=(kc == 7))
                    k.copy("act", VV[:, tt, :], p1[:, 0:128])
                for hh in range(2):
                    head = 2 * hp + hh
                    pb = hh * 64
                    k.dma("sp", TB, V(g.nabias.ap[i, head], g.nabias.bufs))
                    for kt in range(16):
                        j0 = max(0, kt - 3)
                        j1 = min(15, kt + 3)
                        nq = j1 - j0 + 1
                        S = pS[kt % 2]
                        c = 0
                        while c < nq * 128:
                            w = min(512, nq * 128 - c)
                            k.mm(S[:, c:c + w], KT[pb:pb + 64, kt * 128:(kt + 1) * 128],
                                 QT[pb:pb + 64, j0 * 128 + c:j0 * 128 + c + w])
                            c += w
                        u0 = 2 * (j0 - kt) + 7
                        k.stt("dve", stmp[:, 0:nq * 128], S[:, 0:nq * 128], 0.125,
                              TB[:, u0:u0 + 2 * nq, :].re("p u c -> p (u c)"), ALU.mult, ALU.add)
                        k.act(EL[:, kt, 0:nq * 128], stmp[:, 0:nq * 128], AF.Exp)
                        for j in range(j0, j1 + 1):
                            sl = EL[:, kt, (j - j0) * 128:(j - j0 + 1) * 128]
                            ci = g.na_cls.get((kt, j))
                            if ci is None:
                                continue
                            k.tt("pool", sl, sl, nmask[:, ci, :], ALU.mult)
                    for kc_ in range(2):
                        for qb in range(4):
                            S = pS[(kc_ * 4 + qb) % 2]
                            k.mm(S[:, 0:512], KT[pb:pb + 64, (16 + kc_) * 128:(17 + kc_) * 128], QT[pb:pb + 64, qb * 512:(qb + 1) * 512])
                            k.act(EC[:, kc_, qb * 512:(qb + 1) * 512], S[:, 0:512], AF.Exp, scale=0.125)
                    for j in range(16):
                        terms = []
                        for kt in range(max(0, j - 3), min(15, j + 3) + 1):
                            if g.na_cls.get((kt, j)) is None:
                                continue
                            j0 = max(0, kt - 3)
                            terms.append((kt, EL[:, kt, (j - j0) * 128:(j - j0 + 1) * 128]))
                        for kc_ in range(2):
                            terms.append((16 + kc_, EC[:, kc_, j * 128:(j + 1) * 128]))
                        for ti, (kt, Ev) in enumerate(terms):
                            k.mm(pnum, VV[:, kt, :], Ev, start=(ti == 0), stop=(ti == len(terms) - 1))
                        for ti, (kt, Ev) in enumerate(terms):
                            k.mm(pden, onesb, Ev, start=(ti == 0), stop=(ti == len(terms) - 1))
                        k.recip(rden, pden)
                        k.tt("dve", oc[pb:pb + 64, j * 128:(j + 1) * 128], pnum[pb:pb + 64, :], rden[pb:pb + 64, :], ALU.mult)
                    if need_ctx:
                        S = pS[0]
                        for kc_ in range(2):
                            k.mm(S[:, kc_ * 256:(kc_ + 1) * 256], KT[pb:pb + 64, (16 + kc_) * 128:(17 + kc_) * 128], QT[pb:pb + 64, 2048:2304])
                        k.act(EL[:, 0, 0:512], S[:, 0:512], AF.Exp, scale=0.125)
                        for half in range(2):
                            for kc_ in range(2):
                                k.mm(pnum, VV[:, 16 + kc_, :], EL[:, 0, kc_ * 256 + half * 128:kc_ * 256 + (half + 1) * 128],
                                     start=(kc_ == 0), stop=(kc_ == 1))
                            for kc_ in range(2):
                                k.mm(pden, onesb, EL[:, 0, kc_ * 256 + half * 128:kc_ * 256 + (half + 1) * 128],
                                     start=(kc_ == 0), stop=(kc_ == 1))
                            k.recip(rden, pden)
                            k.tt("dve", oc[pb:pb + 64, 2048 + half * 128:2048 + (half + 1) * 128], pnum[pb:pb + 64, :],
                                 rden[pb:pb + 64, :], ALU.mult)
                out_proj_chunk(g, W["od_w_out"], i, 512 + hp * 128, oc, ntl, hp)


def tri_tables():
    s = np.arange(64)[:, None]
    t = np.arange(64)[None, :]
    SU = (s < t).astype(np.float32)
    IU = (s <= t).astype(np.float32)
    SL = (s > t).astype(np.float32)
    IL = (s >= t).astype(np.float32)
    tri = np.stack([SU, IU, SL, IL, -IU, -IL], 0)
    c = np.float32(math.exp(-0.5))
    trif = np.stack([np.concatenate([IU, SU], 1), np.concatenate([IL, SL], 1)], 0) * (-c)
    return tri.astype(np.float32), trif.astype(np.float32)


def even_mixer(g, l, need_ctx):
    k = g.k
    W = g.W
    i = l // 2
    win = W["ev_w_in"]
    hTv = g.hTv
    id64 = g.ident[0:64, 0:64]

    def bc3(v, n):
        return v.re("p (o s) -> p o s", o=1).bc([64, n, 64])

    def bcl(v, n, m=64):
        return v.re("p (c o) -> p c o", o=1).bc([64, n, m])

    with k.scope():
        wo1 = k.sb("wo0", [128, 1024], BF16)
        g.wo_t = [wo1, wo1]
        g.otmp_t = [k.sb("otmp%d" % j, [128, 512], F32) for j in range(2)]
        g.po_t = [k.ps("po%d" % j, [128, 512]) for j in range(2)]
        oTc1 = k.sb("oTc0", [128, TOK], BF16)
        oTc = [oTc1, oTc1]
        pw = k.ps("pw", [128, 1024])
        pd = [k.ps("pd%d" % j, [128, 512]) for j in range(2)]
        pch = [k.ps("pch%d" % j, [128, 512]) for j in range(2)]
        g.pcv = []
        for d in range(2):
            bRU = Buf("pcRU%d" % d)
            bYA = Buf("pcYA%d" % d)
            g.pcv.append([V(pch[d].ap[0:64, 0:64], [bRU]), V(pch[d].ap[0:64, 64:128], [bRU]),
                          V(g.po_t[d].ap[0:64, 0:64], [bYA] + list(g.po_t[d].bufs)),
                          V(g.po_t[d].ap[0:64, 64:128], [bYA] + list(g.po_t[d].bufs))])

        import os as _os
        dbg = _os.environ.get("EVDBG", "")
        with k.scope():
            cw = k.sb("cw", [128, 4, 3], F32)
            for fc_ in range(4):
                k.dma("sp", cw[:, fc_, :], V(W["ev_conv_w"].ap[i, :, fc_ * 128:(fc_ + 1) * 128].rearrange("j p -> p j"), W["ev_conv_w"].bufs),
                      allow_slow_non_contiguous=True)
            wc = k.sb("wc", [128, 8, 384], BF16)
            ux = k.sb("ux", [128, 2050], F32)
            uc = k.sb("uc", [128, 258], F32)
            bg = k.sb("bg", [128, TOK], BF16)
            cx = k.sb("cx", [128, 512], F32)
            yc_ = k.sb("ycv", [128, 2048], F32)
            k.memset("dve", ux, 0.0)
            k.memset("dve", uc, 0.0)
            for fc in range(0 if "noconv" in dbg else 4):
                oc = oTc[fc % 2]
                for j in range(3):
                    c0 = 1920 + j * 512 + fc * 128
                    k.dma("pool", wc[:, :, j * 128:(j + 1) * 128],
                          V(win.ap[i, :, c0:c0 + 128].rearrange("(kc p) c -> p kc c", p=128), win.bufs))
                for bi in range(5):
                    t0 = bi * 512
                    n = 512 if bi < 4 else 256
                    pB, pC, pX = pd[0], pd[1], pch[0]
                    for (pp_, j) in ((pB, 0), (pC, 1), (pX, 2)):
                        for kc in range(8):
                            k.mm(pp_[:, 0:n], wc[:, kc, j * 128:(j + 1) * 128], hTv(kc, t0, t0 + n), start=(kc == 0), stop=(kc == 7))
                    k.copy("act", bg[:, t0:t0 + n], pB[:, 0:n])
                    k.copy("act", cx[:, 0:n], pC[:, 0:n])
                    dst = ux[:, 1 + t0:1 + t0 + n] if bi < 4 else uc[:, 1:257]
                    k.tt("dve", dst, cx[:, 0:n], pX[:, 0:n], ALU.mult)
                for (u, n, t0) in ((ux, 2048, 0), (uc, 256, 2048)):
                    y = yc_[:, 0:n]
                    k.ts("dve", y, u[:, 0:n], cw[:, fc, 0:1], ALU.mult)
                    k.stt("dve", y, u[:, 1:n + 1], cw[:, fc, 1:2], y, ALU.mult, ALU.add)
                    k.stt("dve", y, u[:, 2:n + 2], cw[:, fc, 2:3], y, ALU.mult, ALU.add)
                    k.tt("pool", oc[:, t0:t0 + n], y, bg[:, t0:t0 + n], ALU.mult)
                out_proj_chunk(g, W["ev_w_out"], i, 512 + fc * 128, oc, NT, fc)

        with k.scope():
            msk = k.sb("msk", [64, 6, 64], BF16)
            k.dma("pool", msk, V(g.tri.ap.rearrange("m s t -> s m t"), g.tri.bufs))
            trif = k.sb("trif", [64, 2, 128], F32)
            k.dma("sp", trif, V(g.trif.ap.rearrange("d s t -> s d t"), g.trif.bufs))
            ones64 = k.sb("ones64", [64, 64], BF16)
            k.memset("dve", ones64, 1.0)
            I8 = k.sb("I8", [64, 8, 64], BF16)
            for c_ in range(8):
                k.copy("pool", I8[:, c_, :], id64)
            wlo = k.sb("wlo", [128, 8, 384], BF16)
            k.dma("pool", wlo, V(win.ap[i, :, 1536:1920].rearrange("(kc p) c -> p kc c", p=128), win.bufs))
            wrkv = k.sb("wrkv", [128, 8, 192], BF16)
            wup = k.sb("wup", [64, 2, 64], BF16)
            aup = k.sb("aup", [64, 2, 64], BF16)
            gup = k.sb("gup", [128, 64], BF16)
            pv = k.sb("pvec", [64, 8], F32)
            rowv = k.sb("rowv", [64, 5, 64], F32)
            Yacc = k.sb("Yacc", [64, 36, 64], BF16)
            ot2 = k.sb("ot2", [64, 8, 128], BF16)
            k.memset("dve", ot2, 0.0)
            rTf = k.sb("rTf", [64, 512], BF16)
            krf = k.sb("krf", [64, 512], BF16)
            tA = k.sb("tA", [64, 512], F32)
            tB = k.sb("tB", [64, 512], F32)
            kkf = k.sb("kkf", [64, 512], F32)
            af = k.sb("af", [64, 512], BF16)
            sqb = k.sb("sqb", [64, 512], BF16)
            twT = k.sb("twT", [64, 512], BF16)
            adT = k.sb("adT", [64, 512], BF16)
            sg = k.sb("sg", [64, 8, 64], F32)
            Ep = k.sb("Ep", [64, 8, 64], F32)
            Em = k.sb("Em", [64, 8, 64], BF16)
            Ex = k.sb("Ex", [64, 8, 64], BF16)
            btT = k.sb("btT", [64, 8, 64], BF16)
            ktT = k.sb("ktT", [64, 8, 64], BF16)
            LbT = k.sb("LbT", [64, 8, 64], BF16)
            Lb = k.sb("Lb", [64, 8, 64], BF16)
            Xa = [k.sb("Xa%d" % j, [64, 8, 64], BF16) for j in range(2)]
            XTa = [k.sb("XTa%d" % j, [64, 8, 64], BF16) for j in range(2)]
            Fd = k.sb("Fd", [64, 8, 64], BF16)
            Yd = [k.sb("Yd%d" % j, [64, 8, 64], BF16) for j in range(2)]
            KR = [k.sb("KR%d" % d, [64, 8, 2, 64], BF16) for d in range(2)]
            LkT = [k.sb("LkT%d" % d, [64, 8, 64], BF16) for d in range(2)]
            MkT = [k.sb("MkT%d" % d, [64, 8, 64], BF16) for d in range(2)]
            MbTn = [k.sb("MbTn%d" % d, [64, 8, 64], BF16) for d in range(2)]
            TT = [k.sb("TT%d" % d, [64, 8, 64], BF16) for d in range(2)]
            ktok = [k.sb("ktok%d" % d, [64, 8, 64], BF16) for d in range(2)]
            btokn = [k.sb("btokn%d" % d, [64, 8, 64], BF16) for d in range(2)]
            Vt = [k.sb("Vt%d" % d, [64, 8, 64], BF16) for d in range(2)]
            PCt = [k.sb("PCt%d" % d, [64, 8], F32) for d in range(2)]
            Ast = [[k.sb("Ast%d_%d" % (d, j), [64, 64], BF16) for j in range(2)] for d in range(2)]
            Rsb = [k.sb("Rsb%d" % d, [64, 64], BF16) for d in range(2)]
            Usb = [k.sb("Usb%d" % d, [64, 64], BF16) for d in range(2)]
            rkvt = k.sb("rkvt", [64, 8, 192], BF16)
            sgT = k.sb("sgT", [128, 512], BF16)
            ycn = tA.re("p (c s) -> p c s", s=64)
            ysq = tB.re("p (c s) -> p c s", s=64)
            rsm = k.sb("rsm", [64, 4, 8], F32)

            MS_T = [msk[:, 0, :], msk[:, 2, :]]
            MI_T = [msk[:, 1, :], msk[:, 3, :]]
            nMI_T = [msk[:, 4, :], msk[:, 5, :]]
            MS = [msk[:, 2, :], msk[:, 0, :]]

            def vec_col(name, c0, j):
                src = W[name]
                k.dma("sp", pv[:, j:j + 1], V(src.ap[i:i + 1, c0:c0 + 64].rearrange("o p -> p o"), src.bufs),
                      allow_slow_non_contiguous=True)

            batches_f = [(2048, 4, 32), (0, 8, 0), (512, 8, 8), (1024, 8, 16), (1536, 8, 24)]
            batches_b = [(2048, 4, 32), (1536, 8, 24), (1024, 8, 16), (512, 8, 8), (0, 8, 0)]

            EVH = int(_os.environ.get("EVH", "8")); EVB = int(_os.environ.get("EVB", "5")); EVP = int(_os.environ.get("EVP", "99"))
            EVCHAIN = int(_os.environ.get("EVCHAIN", "1")); EVREAD = int(_os.environ.get("EVREAD", "1"))
            for h in range(0 if "norwkv" in dbg else EVH):
                oc = oTc[(h // 2) % 2]
                hoff = (h % 2) * 64
                c_r = h * 64
                for j, cbase in enumerate((0, 512, 1024)):
                    k.dma("pool", wrkv[:, :, j * 64:(j + 1) * 64],
                          V(win.ap[i, :, cbase + c_r:cbase + c_r + 64].rearrange("(kc p) c -> p kc c", p=128), win.bufs))
                for d in range(2):
                    k.dma("pool", wup[:, d, :], V(W["ev_decay_up"].ap[i, d, :, c_r:c_r + 64], W["ev_decay_up"].bufs))
                    k.dma("pool", aup[:, d, :], V(W["ev_iclr_up"].ap[i, d, :, c_r:c_r + 64], W["ev_iclr_up"].bufs))
                k.dma("pool", gup, V(W["ev_gate_up"].ap[i, :, c_r:c_r + 64], W["ev_gate_up"].bufs))
                vec_col("ev_k_k", c_r, 0)
                vec_col("ev_k_a", c_r, 1)
                for d in range(2):
                    src = W["ev_iclr_a0"]
                    k.dma("sp", pv[:, 3 + d:4 + d], V(src.ap[i, d:d + 1, c_r:c_r + 64].rearrange("o p -> p o"), src.bufs),
                          allow_slow_non_contiguous=True)
                    src = W["ev_decay_w0"]
                    k.dma("sp", rowv[:, d, :], V(src.ap[i, d:d + 1, c_r:c_r + 64].partition_broadcast(64), src.bufs))
                for j, nm in ((2, "ev_r_k"), (3, "ev_lnx_w"), (4, "ev_lnx_b")):
                    src = W[nm]
                    k.dma("sp", rowv[:, j, :], V(src.ap[i:i + 1, c_r:c_r + 64].partition_broadcast(64), src.bufs))
                k.ts("dve", pv[:, 2:3], pv[:, 1:2], -1.0, ALU.mult, 1.0, ALU.add)
                k.memset("dve", Yacc, 0.0)
                for d in range(2):
                    k.memset("dve", Ast[d][0], 0.0)
                stp = [0, 0]

                for bi in range(EVB):
                    for d in range(2):
                        t0, nch, cg0 = (batches_f if d == 0 else batches_b)[bi]
                        n = nch * 64
                        pq, pk_ = pd[0], pd[1]
                        if EVP < 1:
                            continue
                        for kc in range(8):
                            k.mm(pq[0:64, 0:n], wrkv[:, kc, 0:64], hTv(kc, t0, t0 + n), start=(kc == 0), stop=(kc == 7))
                        for kc in range(8):
                            k.mm(pk_[0:64, 0:n], wrkv[:, kc, 64:128], hTv(kc, t0, t0 + n), start=(kc == 0), stop=(kc == 7))
                        k.copy("act", rTf[:, 0:n], pq[0:64, 0:n])
                        k.copy("act", krf[:, 0:n], pk_[0:64, 0:n])
                        k.ts("dve", tA[:, 0:n], krf[:, 0:n], pv[:, 0:1], ALU.mult)
                        k.act(sqb[:, 0:n], tA[:, 0:n], AF.Square)
                        k.mm(pw[0:64, 0:n], ones64, sqb[:, 0:n])
                        k.ts("dve", tB[:, 0:n], pw[0:64, 0:n], 1e-24, ALU.max)
                        k.emit("act", lambda h_: h_.sqrt(out=tB[:, 0:n].ap, in_=tB[:, 0:n].ap), reads=[tB], writes=[tB])
                        k.recip(tB[:, 0:n], tB[:, 0:n])
                        k.tt("dve", kkf[:, 0:n], tA[:, 0:n], tB[:, 0:n], ALU.mult)
                        if EVP < 2:
                            continue
                        for kc in range(8):
                            k.mm(pq[0:64, 0:n], wlo[:, kc, d * 64:(d + 1) * 64], hTv(kc, t0, t0 + n), start=(kc == 0), stop=(kc == 7))
                        for kc in range(8):
                            k.mm(pk_[0:64, 0:n], wlo[:, kc, 128 + d * 64:128 + (d + 1) * 64], hTv(kc, t0, t0 + n), start=(kc == 0), stop=(kc == 7))
                        k.act(twT[:, 0:n], pq[0:64, 0:n], AF.Tanh)
                        k.copy("act", adT[:, 0:n], pk_[0:64, 0:n])
                        k.mm(pq[0:64, 0:n], aup[:, d, :], adT[:, 0:n])
                        k.act(af[:, 0:n], pq[0:64, 0:n], AF.Sigmoid, bias=pv[:, 3 + d:4 + d])
                        if EVP < 3:
                            continue
                        plw = V(pw.ap[0:64, 0:512].rearrange("p (c s) -> p c s", s=64), pw.bufs)
                        for c in range(nch):
                            k.mm(plw[:, c, :], twT[:, c * 64:(c + 1) * 64], wup[:, d, :])
                        k.tt("dve", sg[:, 0:nch, :], plw[:, 0:nch, :], bc3(rowv[:, d, :], nch), ALU.add)
                        k.act(sg[:, 0:nch, :], sg[:, 0:nch, :], AF.Sigmoid)
                        pcum = V(pw.ap[0:64, :].rearrange("p (c s) -> p c s", s=128), pw.bufs)
                        for c in range(nch):
                            k.mm(pcum[:, c, :], sg[:, c, :], trif[:, d, :])
                        k.act(Ep[:, 0:nch, :], pcum[:, 0:nch, 0:64], AF.Exp)
                        k.act(Em[:, 0:nch, :], pcum[:, 0:nch, 0:64], AF.Exp, scale=-1.0)
                        k.act(Ex[:, 0:nch, :], pcum[:, 0:nch, 64:128], AF.Exp)
                        tl = 63 if d == 0 else 0
                        k.copy("dve", PCt[d][:, 0:nch], Ep[:, 0:nch, tl])
                        r3 = lambda v: v[:, 0:n].re("p (c s) -> p c s", s=64)
                        k.tt("dve", KR[d][:, 0:nch, 0, :], r3(kkf), Ex[:, 0:nch, :], ALU.mult)
                        k.tt("pool", KR[d][:, 0:nch, 1, :], r3(rTf), Ep[:, 0:nch, :], ALU.mult)
                        k.tt("pool", tB[:, 0:n], af[:, 0:n], kkf[:, 0:n], ALU.mult)
                        k.tt("pool", btT[:, 0:nch, :], r3(tB), Em[:, 0:nch, :], ALU.mult)
                        k.ts("dve", tA[:, 0:n], af[:, 0:n], pv[:, 1:2], ALU.mult, pv[:, 2:3], ALU.add)
                        k.tt("dve", tA[:, 0:n], tA[:, 0:n], krf[:, 0:n], ALU.mult)
                        k.tt("dve", ktT[:, 0:nch, :], r3(tA), Em[:, 0:nch, :], ALU.mult)
                        if EVP < 4:
                            continue
                        pvv = V(pd[0].ap[0:64, :].rearrange("p (c s) -> p c s", s=64), pd[0].bufs)
                        for c in range(nch):
                            for kc in range(8):
                                k.mm(pvv[:, c, :], hTv(kc, t0 + c * 64, t0 + (c + 1) * 64), wrkv[:, kc, 128:192],
                                     start=(kc == 0), stop=(kc == 7))
                        k.copy("act", Vt[d][:, 0:nch, :], pvv[:, 0:nch, :])
                        if EVP < 5:
                            continue
                        for c in range(nch):
                            k.mm(pcum[:, c, :], btT[:, c, :], KR[d][:, c, :, :].re("p a s -> p (a s)"))
                        k.tt("dve", LbT[:, 0:nch, :], pcum[:, 0:nch, 0:64], bc3(MS_T[d], nch), ALU.mult)
                        k.tt("dve", MbTn[d][:, 0:nch, :], pcum[:, 0:nch, 64:128], bc3(nMI_T[d], nch), ALU.mult)
                        for c in range(nch):
                            k.mm(pcum[:, c, :], ktT[:, c, :], KR[d][:, c, :, :].re("p a s -> p (a s)"))
                        k.tt("dve", LkT[d][:, 0:nch, :], pcum[:, 0:nch, 0:64], bc3(MS_T[d], nch), ALU.mult)
                        k.tt("dve", MkT[d][:, 0:nch, :], pcum[:, 0:nch, 64:128], bc3(MI_T[d], nch), ALU.mult)
                        pl3 = V(pd[1].ap[0:64, :].rearrange("p (c s) -> p c s", s=64), pd[1].bufs)
                        for c in range(nch):
                            k.mm(pl3[:, c, :], KR[d][:, c, 0, :], btT[:, c, :])
                        k.tt("dve", Lb[:, 0:nch, :], pl3[:, 0:nch, :], bc3(MS[d], nch), ALU.mult)
                        if EVP < 6:
                            continue
                        ptr = V(pd[0].ap[0:64, :].rearrange("p (c s) -> p c s", s=64), pd[0].bufs)
                        for c in range(nch):
                            k.mm(ptr[:, c, :], ktT[:, c, :], id64)
                        k.copy("act", ktok[d][:, 0:nch, :], ptr[:, 0:nch, :])
                        for c in range(nch):
                            k.mm(ptr[:, c, :], btT[:, c, :], id64)
                        k.ts("dve", btokn[d][:, 0:nch, :], ptr[:, 0:nch, :], -1.0, ALU.mult)
                        if EVP < 7:
                            continue
                        X, XT = Lb, LbT
                        k.tt("pool", Yd[0][:, 0:nch, :], I8[:, 0:nch, :], LbT[:, 0:nch, :], ALU.subtract)
                        ycur = 0
                        for lev in range(int(_os.environ.get("EVLEV", "5"))):
                            pX = V(pd[0].ap[0:64, :].rearrange("p (c s) -> p c s", s=64), pd[0].bufs)
                            pXT = V(pd[1].ap[0:64, :].rearrange("p (c s) -> p c s", s=64), pd[1].bufs)
                            pY = V(pw.ap[0:64, 0:512].rearrange("p (c s) -> p c s", s=64), pw.bufs)
                            for c in range(nch):
                                k.mm(pX[:, c, :], XT[:, c, :], X[:, c, :])
                            if lev < 4:
                                for c in range(nch):
                                    k.mm(pXT[:, c, :], X[:, c, :], XT[:, c, :])
                            k.copy("act", Xa[lev % 2][:, 0:nch, :], pX[:, 0:nch, :])
                            if lev < 4:
                                k.copy("act", XTa[lev % 2][:, 0:nch, :], pXT[:, 0:nch, :])
                            k.tt("pool", Fd[:, 0:nch, :], Xa[lev % 2][:, 0:nch, :], I8[:, 0:nch, :], ALU.add)
                            for c in range(nch):
                                k.mm(pY[:, c, :], Fd[:, c, :], Yd[ycur][:, c, :])
                            if lev < 4:
                                k.copy("act", Yd[1 - ycur][:, 0:nch, :], pY[:, 0:nch, :])
                                ycur = 1 - ycur
                                X, XT = Xa[lev % 2], XTa[lev % 2]
                            else:
                                k.copy("act", TT[d][:, 0:nch, :], pY[:, 0:nch, :])
                    nchs = [batches_f[bi][1], batches_b[bi][1]]
                    for step in range(max(nchs) if EVCHAIN else 0):
                        for d in range(2):
                            t0, nch, cg0 = (batches_f if d == 0 else batches_b)[bi]
                            if step >= nch:
                                continue
                            c = step if d == 0 else nch - 1 - step
                            cg = cg0 + c
                            A = Ast[d][stp[d] % 2]
                            An = Ast[d][(stp[d] + 1) % 2]
                            stp[d] += 1
                            pc = pch[d]
                            pR = g.pcv[d][0]; pU = g.pcv[d][1]; pY_ = g.pcv[d][2]; pA = g.pcv[d][3]
                            k.mm(pR, KR[d][:, c, 0, :], A, start=True, stop=False)
                            k.mm(pR, LkT[d][:, c, :], Vt[d][:, c, :], start=False, stop=True)
                            k.copy("act", Rsb[d], pR)
                            k.mm(pU, TT[d][:, c, :], Rsb[d])
                            k.copy("act", Usb[d], pU)
                            k.mm(pY_, KR[d][:, c, 1, :], A, start=True, stop=False)
                            k.mm(pY_, MkT[d][:, c, :], Vt[d][:, c, :], start=False, stop=False)
                            k.mm(pY_, MbTn[d][:, c, :], Usb[d], start=False, stop=True)
                            k.mm(pA, id64, A, start=True, stop=False)
                            k.mm(pA, ktok[d][:, c, :], Vt[d][:, c, :], start=False, stop=False)
                            k.mm(pA, btokn[d][:, c, :], Usb[d], start=False, stop=True)
                            k.tt("dve", Yacc[:, cg, :], Yacc[:, cg, :], pY_, ALU.add)
                            k.ts("dve", An, pA, PCt[d][:, c:c + 1], ALU.mult)

                for (t0, nch, cg0) in (batches_f if EVREAD else []):
                    n = nch * 64
                    psg = pd[0]
                    for kc in range(8):
                        k.mm(psg[:, 0:n], wlo[:, kc, 256:384], hTv(kc, t0, t0 + n), start=(kc == 0), stop=(kc == 7))
                    k.act(sgT[:, 0:n], psg[:, 0:n], AF.Sigmoid)
                    prk = V(pw.ap[0:64, :].rearrange("p (c s) -> p c s", s=256), pw.bufs)
                    for half in range((nch + 3) // 4):
                        for c4 in range(4):
                            c = half * 4 + c4
                            for kc in range(8):
                                k.mm(prk[:, c4, 0:192], hTv(kc, t0 + c * 64, t0 + (c + 1) * 64), wrkv[:, kc, :],
                                     start=(kc == 0), stop=(kc == 7))
                        k.copy("act", rkvt[:, half * 4:half * 4 + 4, :], prk[:, 0:4, 0:192])
                    pg = V(pd[1].ap[0:64, :].rearrange("p (c s) -> p c s", s=64), pd[1].bufs)
                    for c in range(nch):
                        k.mm(pg[:, c, :], sgT[:, c * 64:(c + 1) * 64], gup)
                    y = Yacc[:, cg0:cg0 + nch, :]
                    s1, mu, s2, rk = (rsm[:, j, 0:nch] for j in range(4))
                    k.reduce("dve", s1, y, ALU.add)
                    k.ts("dve", mu, s1, 1.0 / 64, ALU.mult)
                    k.tt("dve", ycn[:, 0:nch, :], y, bcl(mu, nch), ALU.subtract)
                    k.act(ysq[:, 0:nch, :], ycn[:, 0:nch, :], AF.Square)
                    k.reduce("dve", s2, ysq[:, 0:nch, :], ALU.add)
                    k.rsqrt(s2, s2, 1.0 / 64, 64e-5)
                    k.tt("dve", ycn[:, 0:nch, :], ycn[:, 0:nch, :], bcl(s2, nch), ALU.mult)
                    k.tt("dve", ycn[:, 0:nch, :], ycn[:, 0:nch, :], bc3(rowv[:, 3, :], nch), ALU.mult)
                    k.tt("dve", ycn[:, 0:nch, :], ycn[:, 0:nch, :], bc3(rowv[:, 4, :], nch), ALU.add)
                    k.tt("pool", ysq[:, 0:nch, :], rkvt[:, 0:nch, 0:64], rkvt[:, 0:nch, 64:128], ALU.mult)
                    k.tt("pool", ysq[:, 0:nch, :], ysq[:, 0:nch, :], bc3(rowv[:, 2, :], nch), ALU.mult)
                    k.reduce("dve", rk, ysq[:, 0:nch, :], ALU.add)
                    k.tt("dve", ysq[:, 0:nch, :], rkvt[:, 0:nch, 128:192], bcl(rk, nch), ALU.mult)
                    k.tt("dve", ycn[:, 0:nch, :], ycn[:, 0:nch, :], ysq[:, 0:nch, :], ALU.add)
                    k.tt("dve", ot2[:, 0:nch, hoff:hoff + 64], ycn[:, 0:nch, :], pg[:, 0:nch, :], ALU.mult)
                    ptr2 = V(pd[0].ap[:, :].rearrange("p (c s) -> p c s", s=64), pd[0].bufs)
                    for c in range(nch):
                        k.mm(ptr2[:, c, :], ot2[:, c, :], id64)
                    k.copy("act", oc[hoff:hoff + 64, t0:t0 + n], ptr2[hoff:hoff + 64, 0:nch, :].re("p c s -> p (c s)"))
                if h % 2 == 1:
                    out_proj_chunk(g, W["ev_w_out"], i, (h // 2) * 128, oc, NT, h // 2)


_CACHE = {}


def _get_program(**kw):
    key = tuple(sorted(kw.items()))
    if key not in _CACHE:
        _CACHE[key] = build_program(**kw)
    return _CACHE[key]


def make_in_maps(inputs, cores):
    f = lambda a: np.ascontiguousarray(np.asarray(a, dtype=np.float32))
    shared = {}
    for name, shape in WSPECS:
        shared[name] = f(inputs[name]).reshape(shape)
    nam, _ = na_tables()
    shared["namask"] = nam
    shared["nabias"] = na_bias_layout(shared["od_rel_bias"])
    shared["ropec"], shared["ropes"] = rope_tables()
    shared["tri"], shared["trif"] = tri_tables()
    maps = []
    for b in cores:
        m = dict(shared)
        m["xin"] = np.ascontiguousarray(np.concatenate([f(inputs["x"][b]), f(inputs["ctx"][b])], axis=0))
        m["cc"] = np.ascontiguousarray(np.stack([f(inputs["c"][b]), f(inputs["c_ctx"])], axis=0))
        maps.append(m)
    return maps


def kernel(**inputs):
    k = _get_program()
    maps = make_in_maps(inputs, list(range(8)))
    res = run_bass_kernel_spmd(k.nc, maps, core_ids=list(range(8)))
    return np.stack([res.results[b]["out"] for b in range(8)], axis=0).astype(np.float32)
```

```python
import contextlib
import math
import numpy as np
import concourse.bass as bass
import concourse.mybir as mybir
from concourse.bass_utils import run_bass_kernel_spmd

F32 = mybir.dt.float32
BF16 = mybir.dt.bfloat16
I32 = mybir.dt.int32
U32 = mybir.dt.uint32
AF = mybir.ActivationFunctionType
ALU = mybir.AluOpType
AX = mybir.AxisListType


class Buf:
    __slots__ = ("name", "writer", "readers")

    def __init__(self, name):
        self.name = name
        self.writer = None
        self.readers = []


class V:
    __slots__ = ("ap", "bufs")

    def __init__(self, ap, bufs):
        self.ap = ap
        self.bufs = tuple(bufs)

    def __getitem__(self, key):
        return V(self.ap[key], self.bufs)

    def re(self, pattern_, **kw):
        return V(self.ap.rearrange(pattern_, **kw), self.bufs)

    def bc(self, shape):
        return V(self.ap.to_broadcast(list(shape)), self.bufs)

    def with_ap(self, ap):
        return V(ap, self.bufs)


class Eng:
    def __init__(self, name, h, sem):
        self.name = name
        self.h = h
        self.sem = sem
        self.count = 0
        self.known = {}
        self.hist = []


class K:
    def __init__(self):
        self.nc = bass.Bass("TRN2", target_bir_lowering=False)
        self.es = contextlib.ExitStack()
        self.root = contextlib.ExitStack()
        self.uid = 0
        nc = self.nc
        self.engs = {}
        for name, h in (("pe", nc.tensor), ("act", nc.scalar), ("dve", nc.vector),
                        ("pool", nc.gpsimd), ("sp", nc.sync)):
            sem = self.root.enter_context(nc.semaphore("s_" + name))
            self.engs[name] = Eng(name, h, sem)
        self.dma_sems = {}
        self.nbuf = 0
        self.same_eng_sync = True
        self.n_inst = 0
        self.n_wait = 0

    def sb(self, name, shape, dtype, nsplit=None):
        self.uid += 1
        t = self.es.enter_context(self.nc.sbuf_tensor("%s_%d" % (name, self.uid), list(shape), dtype))
        return V(t[:], [Buf(name)])

    def ps(self, name, shape, dtype=F32):
        self.uid += 1
        t = self.es.enter_context(self.nc.psum_tensor("%s_%d" % (name, self.uid), list(shape), dtype))
        return V(t[:], [Buf(name)])

    def dram(self, name, shape, dtype, kind="Internal"):
        t = self.nc.dram_tensor(name, list(shape), dtype, kind=kind)
        return V(t.ap(), [Buf(name)])

    def sub(self, v, name):
        return V(v.ap, [Buf(name)])

    def _need(self, eng, dep):
        if dep is None:
            return
        if dep[0] == 'e':
            _, en, idx = dep
            if en == eng.name and (en == "pe" or not self.same_eng_sync):
                return
            key = en
            val = idx
        else:
            _, key, val = dep
        if eng.known.get(key, 0) >= val:
            return
        if dep[0] == 'e':
            sem = self.engs[en].sem
        else:
            sem = self.dma_sems[key][0]
        eng.h.wait_ge(sem, val)
        self.n_wait += 1
        nk = dict(eng.known)
        nk[key] = val
        if dep[0] == 'e' and en != eng.name:
            other = self.engs[en].hist[idx - 1]
            for k2, v2 in other.items():
                if nk.get(k2, 0) < v2:
                    nk[k2] = v2
        eng.known = nk

    def emit(self, engname, fn, reads=(), writes=()):
        eng = self.engs[engname]
        for v in reads:
            for b in v.bufs:
                self._need(eng, b.writer)
        for v in writes:
            for b in v.bufs:
                self._need(eng, b.writer)
                for r in b.readers:
                    self._need(eng, r)
        ins = fn(eng.h)
        eng.count += 1
        ins.then_inc(eng.sem, 1)
        me = ('e', engname, eng.count)
        eng.hist.append(eng.known)
        for v in reads:
            for b in v.bufs:
                b.readers.append(me)
        for v in writes:
            for b in v.bufs:
                b.writer = me
                b.readers = []
        self.n_inst += 1
        return ins

    def dma(self, qname, out, in_, **kw):
        eng = self.engs[qname]
        for b in in_.bufs:
            self._need(eng, b.writer)
        for b in out.bufs:
            self._need(eng, b.writer)
            for r in b.readers:
                self._need(eng, r)
        key = out.bufs[0].name
        if key not in self.dma_sems:
            sem = self.root.enter_context(self.nc.semaphore("d_" + key))
            self.dma_sems[key] = [sem, 0]
        ent = self.dma_sems[key]
        ent[1] += 16
        ins = eng.h.dma_start(out=out.ap, in_=in_.ap, **kw)
        ins.then_inc(ent[0], 16)
        me = ('d', key, ent[1])
        for b in in_.bufs:
            b.readers.append(me)
        for b in out.bufs:
            b.writer = me
            b.readers = []
        self.n_inst += 1
        return ins

    def wait_all(self, engname, vs):
        eng = self.engs[engname]
        for v in vs:
            for b in v.bufs:
                self._need(eng, b.writer)

    def mm(self, out, lhsT, rhs, start=True, stop=True):
        rd = [lhsT, rhs]
        return self.emit("pe", lambda h: h.matmul(out.ap, lhsT=lhsT.ap, rhs=rhs.ap, start=start, stop=stop),
                         reads=rd, writes=[out])

    def tr(self, out, in_, ident):
        return self.emit("pe", lambda h: h.transpose(out.ap, in_.ap, ident.ap), reads=[in_, ident], writes=[out])

    def act(self, out, in_, func, bias=None, scale=None, accum_out=None):
        rd = [in_]
        kw = {}
        if bias is not None:
            if isinstance(bias, V):
                rd.append(bias); kw["bias"] = bias.ap
            else:
                kw["bias"] = bias
        if scale is not None:
            if isinstance(scale, V):
                rd.append(scale); kw["scale"] = scale.ap
            else:
                kw["scale"] = scale
        wr = [out]
        if accum_out is not None:
            wr.append(accum_out); kw["accum_out"] = accum_out.ap
        return self.emit("act", lambda h: h.activation(out=out.ap, in_=in_.ap, func=func, **kw), reads=rd, writes=wr)

    def tt(self, e, out, in0, in1, op):
        return self.emit(e, lambda h: h.tensor_tensor(out=out.ap, in0=in0.ap, in1=in1.ap, op=op),
                         reads=[in0, in1], writes=[out])

    def ts(self, e, out, in0, s1, op0, s2=None, op1=None, accum_out=None):
        rd = [in0]
        a1 = s1
        if isinstance(s1, V):
            rd.append(s1); a1 = s1.ap
        a2 = s2
        if isinstance(s2, V):
            rd.append(s2); a2 = s2.ap
        kw = {}
        if op1 is not None:
            kw["op1"] = op1
        wr = [out]
        if accum_out is not None:
            wr.append(accum_out); kw["accum_out"] = accum_out.ap
        return self.emit(e, lambda h: h.tensor_scalar(out=out.ap, in0=in0.ap, scalar1=a1, scalar2=a2, op0=op0, **kw),
                         reads=rd, writes=wr)

    def stt(self, e, out, in0, scalar, in1, op0, op1):
        rd = [in0, in1]
        a = scalar
        if isinstance(scalar, V):
            rd.append(scalar); a = scalar.ap
        return self.emit(e, lambda h: h.scalar_tensor_tensor(out=out.ap, in0=in0.ap, scalar=a, in1=in1.ap, op0=op0, op1=op1),
                         reads=rd, writes=[out])

    def copy(self, e, out, in_):
        if e == "act":
            return self.emit(e, lambda h: h.copy(out=out.ap, in_=in_.ap), reads=[in_], writes=[out])
        return self.emit(e, lambda h: h.tensor_copy(out=out.ap, in_=in_.ap), reads=[in_], writes=[out])

    def memset(self, e, out, val):
        return self.emit(e, lambda h: h.memset(out.ap, val), reads=[], writes=[out])

    def reduce(self, e, out, in_, op, axis=AX.X):
        return self.emit(e, lambda h: h.tensor_reduce(out=out.ap, in_=in_.ap, axis=axis, op=op), reads=[in_], writes=[out])

    def recip(self, out, in_):
        return self.emit("dve", lambda h: h.reciprocal(out=out.ap, in_=in_.ap), reads=[in_], writes=[out])

    def barrier(self):
        for en, e in self.engs.items():
            for en2, e2 in self.engs.items():
                if e2.count > 0 and (en2 != en or en != "pe"):
                    self._need(e, ('e', en2, e2.count))
            for key, (sem, cnt) in self.dma_sems.items():
                self._need(e, ('d', key, cnt))

    @contextlib.contextmanager
    def scope(self):
        old = self.es
        self.es = contextlib.ExitStack()
        self._scope_depth = getattr(self, "_scope_depth", 0) + 1
        try:
            yield
        finally:
            self.barrier()
            self.es.close()
            self.es = old
            self._scope_depth -= 1

    def rsqrt(self, out, in_, mul, add):
        self.ts("dve", out, in_, mul, ALU.mult, add, ALU.add)
        self.emit("act", lambda h: h.sqrt(out=out.ap, in_=out.ap), reads=[out], writes=[out])
        self.recip(out, out)

    def rsqrt_from(self, out, in_, mul, add):
        self.ts("dve", out, in_, mul, ALU.mult, add, ALU.add)
        self.emit("act", lambda h: h.sqrt(out=out.ap, in_=out.ap), reads=[out], writes=[out])
        self.recip(out, out)

    def finish(self, outs):
        sp = self.engs["sp"]
        for v in outs:
            for b in v.bufs:
                self._need(sp, b.writer)
        for en, e in self.engs.items():
            if en != "sp" and e.count > 0:
                self._need(sp, ('e', en, e.count))
        for key, (sem, cnt) in self.dma_sems.items():
            self._need(sp, ('d', key, cnt))


NT = 18
TOK = 2304
D = 1024
RMS_EPS_ = 1e-6

WSPECS = [
    ("ada_w", [4, 1024, 6144]), ("ada_b", [4, 6144]), ("norm_g", [4, 2, 1024]), ("final_g", [1, 1024]),
    ("ev_w_in", [2, 1024, 3456]), ("ev_w_out", [2, 1024, 1024]), ("ev_decay_w0", [2, 2, 512]),
    ("ev_decay_up", [2, 2, 64, 512]), ("ev_iclr_a0", [2, 2, 512]), ("ev_iclr_up", [2, 2, 64, 512]),
    ("ev_gate_up", [2, 128, 512]), ("ev_k_k", [2, 512]), ("ev_k_a", [2, 512]), ("ev_r_k", [2, 512]),
    ("ev_lnx_w", [2, 512]), ("ev_lnx_b", [2, 512]), ("ev_conv_w", [2, 3, 512]),
    ("od_w_in", [2, 1024, 3072]), ("od_w_out", [2, 1024, 1024]), ("od_lambda", [2, 256]),
    ("od_subln", [2, 128]), ("od_rel_bias", [2, 8, 15, 31]),
    ("moe_wg", [4, 1024, 4]), ("moe_bg", [4, 4]), ("moe_we", [4, 1024, 16]), ("moe_be", [4, 16]),
    ("moe_w1", [4, 16, 1024, 512]), ("moe_w3", [4, 16, 1024, 512]), ("moe_w2", [4, 16, 512, 1024]),
]


class G:
    pass


def build_program(layers=(0, 1, 2, 3), do_mixer=True, final=True):
    k = K()
    g = G()
    g.k = k
    W = {}
    xin = k.dram("xin", [TOK, D], F32, "ExternalInput")
    cc = k.dram("cc", [2, D], F32, "ExternalInput")
    for name, shape in WSPECS:
        W[name] = k.dram(name, shape, F32, "ExternalInput")
    out = k.dram("out", [2048, D], F32, "ExternalOutput")
    g.W = W
    nam, g.na_cls = na_tables()
    g.n_na_cls = nam.shape[0]
    g.namask = k.dram("namask", list(nam.shape), F32, "ExternalInput")
    g.nabias = k.dram("nabias", [2, 8, 128, 16, 64], F32, "ExternalInput")
    g.ropec = k.dram("ropec", [128, 2048], F32, "ExternalInput")
    g.ropes = k.dram("ropes", [128, 2048], F32, "ExternalInput")
    g.tri = k.dram("tri", [6, 64, 64], F32, "ExternalInput")
    g.trif = k.dram("trif", [2, 64, 128], F32, "ExternalInput")

    xs = k.sb("xs", [128, NT, D], F32)
    g.xs_t = [V(xs.ap[:, tt, :], [Buf("xs%d" % tt)]) for tt in range(NT)]
    hT = k.sb("hT", [128, 8, TOK], BF16)
    g.hT = hT
    g.hT_bufs = [Buf("hT%d" % tt) for tt in range(NT)]

    def hTv(kc, t0, t1):
        return V(hT.ap[:, kc, t0:t1], g.hT_bufs[t0 // 128:(t1 + 127) // 128])
    g.hTv = hTv

    identf = k.sb("identf", [128, 128], F32)
    ident = k.sb("ident", [128, 128], BF16)
    k.memset("dve", identf, 0.0)
    k.emit("pool", lambda h: h.affine_select(out=identf.ap, in_=identf.ap, pattern=[[-1, 128]],
                                            compare_op=ALU.not_equal, fill=1.0, base=0, channel_multiplier=1),
           reads=[identf], writes=[identf])
    k.copy("dve", ident, identf)
    g.ident = ident
    g.identf = identf

    for tt in range(NT):
        k.dma("sp", g.xs_t[tt], V(xin.ap[tt * 128:(tt + 1) * 128, :], xin.bufs))

    siluL = k.sb("siluL", [128, 2, 8, 128], BF16)
    with k.scope():
        ccT = k.sb("ccT", [128, 2, 8], F32)
        ccS = k.sb("ccS", [128, 2, 8], F32)
        k.dma("sp", ccT, V(cc.ap.rearrange("r (kc p) -> p r kc", p=128), cc.bufs), allow_slow_non_contiguous=True)
        k.act(ccS, ccT, AF.Silu)
        for r in range(2):
            for kc in range(8):
                k.copy("dve", siluL[:, r, kc, :], ccS[:, r, kc:kc + 1].bc([128, 128]))
    g.siluL = siluL

    for l in layers:
        need_ctx = l < 3
        with k.scope():
            g1t = k.sb("g1t", [128, 2, 1024], F32)
            with k.scope():
                mod = k.sb("mod", [128, 2, 3072], F32)
                ada_stage(g, l, 0, mod)
                norm_phase(g, l, 0, mod, NT)
                k.copy("pool", g1t, mod[:, :, 2048:3072])
            g.g1t = g1t
            if do_mixer:
                if l % 2 == 0:
                    even_mixer(g, l, need_ctx)
                else:
                    odd_mixer(g, l, need_ctx)
        with k.scope():
            mod = k.sb("mod", [128, 2, 3072], F32)
            ada_stage(g, l, 1, mod)
            ntl = NT if need_ctx else 16
            norm_phase(g, l, 1, mod, ntl)
            moe_phase(g, l, mod, ntl)

    with k.scope():
        fg = k.sb("fg", [128, D], F32)
        k.dma("sp", fg, V(W["final_g"].ap[0:1, :].partition_broadcast(128), W["final_g"].bufs))
        junk = k.sb("junk", [128, D], BF16)
        ss = k.sb("ss", [128, NT], F32)
        rstd = k.sb("rstd", [128, NT], F32)
        k.memset("dve", ss, 0.0)
        ot = [k.sb("ot%d" % i, [128, D], F32) for i in range(2)]
        for tt in range(16):
            o = ot[tt % 2]
            if final:
                k.act(junk, g.xs_t[tt], AF.Square, accum_out=ss[:, tt:tt + 1])
                k.rsqrt(rstd[:, tt:tt + 1], ss[:, tt:tt + 1], 1.0 / D, RMS_EPS_)
                k.stt("dve", o, g.xs_t[tt], rstd[:, tt:tt + 1], fg, ALU.mult, ALU.mult)
            else:
                k.copy("dve", o, g.xs_t[tt])
            k.dma("sp", V(out.ap[tt * 128:(tt + 1) * 128, :], out.bufs), o)
    k.finish([out])
    return k


def ada_stage(g, l, st, mod):
    k = g.k
    W = g.W
    with k.scope():
        wch = [k.sb("adaw%d" % i, [128, 8, 512], BF16) for i in range(2)]
        bch = [k.sb("adab%d" % i, [128, 512], F32) for i in range(2)]
        pA = [k.ps("adap%d" % i, [128, 512]) for i in range(2)]
        for ci in range(6):
            c0 = st * 3072 + ci * 512
            w = wch[ci % 2]
            b = bch[ci % 2]
            k.dma("pool", w, V(W["ada_w"].ap[l, :, c0:c0 + 512].rearrange("(kc p) c -> p kc c", p=128), W["ada_w"].bufs))
            k.dma("sp", b, V(W["ada_b"].ap[l:l + 1, c0:c0 + 512].partition_broadcast(128), W["ada_b"].bufs))
            for r in range(2):
                p = pA[r]
                for kc in range(8):
                    k.mm(p, g.siluL[:, r, kc, :], w[:, kc, :], start=(kc == 0), stop=(kc == 7))
                k.tt("dve", mod[:, r, ci * 512:(ci + 1) * 512], p, b, ALU.add)


def norm_phase(g, l, st, mod, ntiles):
    k = g.k
    W = g.W
    with k.scope():
        ng = k.sb("ng", [128, D], F32)
        k.dma("sp", ng, V(W["norm_g"].ap[l, st:st + 1, :].partition_broadcast(128), W["norm_g"].bufs))
        for r in range(2):
            k.stt("dve", mod[:, r, 1024:2048], mod[:, r, 1024:2048], 1.0, ng, ALU.add, ALU.mult)
        junk = k.sb("junk", [128, D], BF16)
        ss = k.sb("ss", [128, NT], F32)
        rstd = k.sb("rstd", [128, NT], F32)
        k.memset("dve", ss, 0.0)
        tmp = [k.sb("ntmp%d" % i, [128, D], F32) for i in range(2)]
        hb = [k.sb("hb%d" % i, [128, D], BF16) for i in range(2)]
        pT = [k.ps("pT%d" % i, [128, 8, 128], BF16) for i in range(2)]
        for tt in range(ntiles):
            i = tt % 2
            r = 0 if tt < 16 else 1
            k.act(junk, g.xs_t[tt], AF.Square, accum_out=ss[:, tt:tt + 1])
            k.rsqrt(rstd[:, tt:tt + 1], ss[:, tt:tt + 1], 1.0 / D, RMS_EPS_)
            k.stt("dve", tmp[i], g.xs_t[tt], rstd[:, tt:tt + 1], mod[:, r, 1024:2048], ALU.mult, ALU.mult)
            k.tt("pool", hb[i], tmp[i], mod[:, r, 0:1024], ALU.add)
            for kc in range(8):
                k.tr(pT[i][:, kc, :], hb[i][:, kc * 128:(kc + 1) * 128], g.ident)
            k.copy("act", V(g.hT.ap[:, :, tt * 128:(tt + 1) * 128], [g.hT_bufs[tt]]), pT[i])


def moe_phase(g, l, mod, ntiles):
    k = g.k
    W = g.W
    with k.scope():
        wr = k.sb("wr", [128, 8, 20], BF16)
        k.dma("pool", wr[:, :, 0:4], V(W["moe_wg"].ap[l].rearrange("(kc p) c -> p kc c", p=128), W["moe_wg"].bufs),
              allow_slow_non_contiguous=True)
        k.dma("pool", wr[:, :, 4:20], V(W["moe_we"].ap[l].rearrange("(kc p) c -> p kc c", p=128), W["moe_we"].bufs),
              allow_slow_non_contiguous=True)
        rb = k.sb("rb", [128, 20], F32)
        k.dma("sp", rb[:, 0:4], V(W["moe_bg"].ap[l:l + 1, :].partition_broadcast(128), W["moe_bg"].bufs))
        k.dma("sp", rb[:, 4:20], V(W["moe_be"].ap[l:l + 1, :].partition_broadcast(128), W["moe_be"].bufs))
        comb = k.sb("comb", [128, NT, 16], F32)
        with k.scope():
            pr = [k.ps("pr%d" % i, [128, 20]) for i in range(2)]
            lg = k.sb("lg", [128, 20], F32)
            sm = k.sb("rsm", [128, 16], F32)
            oh = k.sb("oh", [128, 4], F32)
            eg = k.sb("eg", [128, 4], F32)
            t44 = k.sb("t44", [128, 4, 4], F32)
            leg8 = k.sb("leg8", [128, 8], F32)
            m8 = k.sb("m8", [128, 8], F32)
            selm = k.sb("selm", [128, 4], F32)
            ee = k.sb("ee", [128, 4], F32)
            wts = k.sb("wts", [128, 4], F32)
            k.memset("dve", leg8, -1e30)
            k.memset("dve", sm, 0.0)
            for tt in range(ntiles):
                p = pr[tt % 2]
                for kc in range(8):
                    k.mm(p, g.hTv(kc, tt * 128, (tt + 1) * 128), wr[:, kc, :], start=(kc == 0), stop=(kc == 7))
                k.tt("dve", lg, p, rb, ALU.add)
                gmax, ngmax, sg, nm1, s2, den = (sm[:, j:j + 1] for j in range(6))
                k.reduce("dve", gmax, lg[:, 0:4], ALU.max)
                k.ts("dve", oh, lg[:, 0:4], gmax, ALU.is_equal)
                k.ts("dve", ngmax, gmax, -1.0, ALU.mult)
                k.act(eg, lg[:, 0:4], AF.Exp, bias=ngmax)
                k.reduce("dve", sg, eg, ALU.add)
                k.tt("dve", t44, lg[:, 4:20].re("p (g e) -> p g e", g=4),
                     oh.re("p (g o) -> p g o", o=1).bc([128, 4, 4]), ALU.mult)
                k.reduce("dve", leg8[:, 0:4], t44.re("p g e -> p e g"), ALU.add)
                k.emit("dve", lambda h: h.max(out=m8.ap, in_=leg8.ap), reads=[leg8], writes=[m8])
                k.ts("dve", selm, leg8[:, 0:4], m8[:, 1:2], ALU.is_ge)
                k.ts("dve", nm1, m8[:, 0:1], -1.0, ALU.mult)
                k.act(ee, leg8[:, 0:4], AF.Exp, bias=nm1)
                k.tt("dve", ee, ee, selm, ALU.mult)
                k.reduce("dve", s2, ee, ALU.add)
                k.tt("dve", den, s2, sg, ALU.mult)
                k.recip(den, den)
                k.ts("dve", wts, ee, den, ALU.mult)
                k.tt("dve", comb[:, tt, :].re("p (g e) -> p g e", g=4),
                     oh.re("p (g o) -> p g o", o=1).bc([128, 4, 4]),
                     wts.re("p (o e) -> p o e", o=1).bc([128, 4, 4]), ALU.mult)
        with k.scope():
            w1b = [k.sb("w1b%d" % i, [128, 8, 512], BF16) for i in range(2)]
            w3b = [k.sb("w3b%d" % i, [128, 8, 512], BF16) for i in range(2)]
            w2b = [k.sb("w2b%d" % i, [128, 4, 1024], BF16) for i in range(2)]
            pa = [k.ps("pa%d" % i, [128, 512]) for i in range(2)]
            pb = [k.ps("pb%d" % i, [128, 512]) for i in range(2)]
            py = [k.ps("py%d" % i, [128, 512]) for i in range(2)]
            sA = [k.sb("sA%d" % i, [128, 512], BF16) for i in range(2)]
            gt = [[k.sb("gt%d_%d" % (i, fc), [128, 512], BF16) for fc in range(4)] for i in range(2)]
            ytmp = [k.sb("ytmp%d" % i, [128, 512], F32) for i in range(2)]
            blocks = [(b * 512, 512) for b in range(4)]
            if ntiles == NT:
                blocks.append((2048, 256))
            cnt = 0
            for e in range(16):
                s = e % 2
                k.dma("pool", w1b[s], V(W["moe_w1"].ap[l, e].rearrange("(kc p) f -> p kc f", p=128), W["moe_w1"].bufs))
                k.dma("pool", w3b[s], V(W["moe_w3"].ap[l, e].rearrange("(kc p) f -> p kc f", p=128), W["moe_w3"].bufs))
                k.dma("pool", w2b[s], V(W["moe_w2"].ap[l, e].rearrange("(fc p) d -> p fc d", p=128), W["moe_w2"].bufs))
                for bi, (t0, n) in enumerate(blocks):
                    gi = bi % 2
                    for fc in range(4):
                        a = pa[fc % 2]
                        b = pb[fc % 2]
                        for kc in range(8):
                            k.mm(a[:, 0:n], w1b[s][:, kc, fc * 128:(fc + 1) * 128], g.hTv(kc, t0, t0 + n),
                                 start=(kc == 0), stop=(kc == 7))
                        for kc in range(8):
                            k.mm(b[:, 0:n], w3b[s][:, kc, fc * 128:(fc + 1) * 128], g.hTv(kc, t0, t0 + n),
                                 start=(kc == 0), stop=(kc == 7))
                        k.act(sA[fc % 2][:, 0:n], a[:, 0:n], AF.Silu)
                        k.tt("dve", gt[gi][fc][:, 0:n], sA[fc % 2][:, 0:n], b[:, 0:n], ALU.mult)
                    for ti in range(n // 128):
                        tt = t0 // 128 + ti
                        r = 0 if tt < 16 else 1
                        for dh in range(2):
                            y = py[cnt % 2]
                            yt = ytmp[cnt % 2]
                            cnt += 1
                            for fc in range(4):
                                k.mm(y, gt[gi][fc][:, ti * 128:(ti + 1) * 128], w2b[s][:, fc, dh * 512:(dh + 1) * 512],
                                     start=(fc == 0), stop=(fc == 3))
                            k.stt("dve", yt, y, comb[:, tt, e:e + 1], mod[:, r, 2048 + dh * 512:2048 + (dh + 1) * 512],
                                  ALU.mult, ALU.mult)
                            xv = g.xs_t[tt][:, dh * 512:(dh + 1) * 512]
                            k.tt("pool", xv, xv, yt, ALU.add)


def out_proj_chunk(g, wsrc, li, row0, oTc, ntiles, tagi):
    k = g.k
    wo = g.wo_t[tagi % 2]
    k.dma("pool", wo, V(wsrc.ap[li, row0:row0 + 128, :], wsrc.bufs))
    for tt in range(ntiles):
        r = 0 if tt < 16 else 1
        for dh in range(2):
            y = g.po_t[(tt * 2 + dh) % 2]
            yt = g.otmp_t[(tt * 2 + dh) % 2]
            k.mm(y, oTc[:, tt * 128:(tt + 1) * 128], wo[:, dh * 512:(dh + 1) * 512])
            k.tt("dve", yt, y, g.g1t[:, r, dh * 512:(dh + 1) * 512], ALU.mult)
            xv = g.xs_t[tt][:, dh * 512:(dh + 1) * 512]
            k.tt("pool", xv, xv, yt, ALU.add)


def na_tables():
    rows = 32
    def rs(rq):
        return min(max(rq - 4, 0), rows - 8)
    def cs(cq):
        return min(max(cq - 8, 0), 64 - 16)
    colm = np.zeros((64, 64), np.float32)
    for cq in range(64):
        colm[cs(cq):cs(cq) + 16, cq] = 1.0
    classes = []
    keys = {}
    cls = {}
    for kt in range(16):
        for j in range(max(0, kt - 3), min(15, kt + 3) + 1):
            pat = []
            for rkl in range(2):
                for rql in range(2):
                    rk = 2 * kt + rkl
                    rq = 2 * j + rql
                    pat.append(1 if rs(rq) <= rk < rs(rq) + 8 else 0)
            pat = tuple(pat)
            if sum(pat) == 0:
                continue
            if pat not in keys:
                m = np.zeros((128, 128), np.float32)
                for rkl in range(2):
                    for rql in range(2):
                        if pat[rkl * 2 + rql]:
                            m[rkl * 64:(rkl + 1) * 64, rql * 64:(rql + 1) * 64] = colm
                keys[pat] = len(classes)
                classes.append(m)
            cls[(kt, j)] = keys[pat]
    return np.stack(classes, 0), cls


def rope_tables():
    inv = 10000.0 ** (-np.arange(16, dtype=np.float64) / 16.0)
    t = np.arange(2048)
    pos = np.stack([t // 64, t % 64], 0).astype(np.float64)
    cosT = np.zeros((128, 2048), np.float32)
    sinT = np.zeros((128, 2048), np.float32)
    for p in range(128):
        d = p % 64
        ax = 0 if d < 32 else 1
        f = d % 16
        ang = (pos[ax].astype(np.float32) * np.float32(inv[f])).astype(np.float32)
        cosT[p] = np.cos(ang)
        sinT[p] = np.sin(ang)
    return cosT, sinT


def na_bias_layout(rb):
    rkl = np.arange(2)[:, None, None, None]
    ck = np.arange(64)[None, :, None, None]
    u = np.arange(16)[None, None, :, None]
    cq = np.arange(64)[None, None, None, :]
    dr = rkl + 14 - u + 0 * ck + 0 * cq
    dc = ck - cq + 15 + 0 * rkl + 0 * u
    ok = (dr >= 0) & (dr <= 14) & (dc >= 0) & (dc <= 30)
    drc = np.clip(dr, 0, 14)
    dcc = np.clip(dc, 0, 30)
    t = rb[:, :, drc, dcc]
    t = np.where(ok[None, None], t, np.float32(0.0))
    return np.ascontiguousarray(t.reshape(2, 8, 128, 16, 64).astype(np.float32))


def odd_mixer(g, l, need_ctx):
    k = g.k
    W = g.W
    i = l // 2
    lam_init = 0.8 - 0.6 * math.exp(-0.3 * l)
    ntl = NT if need_ctx else 16
    win = W["od_w_in"]
    hTv = g.hTv
    with k.scope():
        g.wo_t = [k.sb("wo%d" % j, [128, 1024], BF16) for j in range(2)]
        g.otmp_t = [k.sb("otmp%d" % j, [128, 512], F32) for j in range(2)]
        onesb = k.sb("onesb", [128, 128], BF16)
        k.memset("dve", onesb, 1.0)
        oTc = [k.sb("oTc%d" % j, [128, TOK], BF16) for j in range(2)]
        QT = k.sb("QT", [128, TOK], BF16)
        KT = k.sb("KT", [128, TOK], BF16)
        VV = k.sb("VV", [128, NT, 128], BF16)
        wq = k.sb("wq", [128, 8, 128], BF16)
        wk = k.sb("wk", [128, 8, 128], BF16)
        wv = k.sb("wv", [128, 8, 128], BF16)
        lam = k.sb("lam", [128, 256], F32)
        k.dma("sp", lam, V(W["od_lambda"].ap[i:i + 1, :].partition_broadcast(128), W["od_lambda"].bufs))
        lsm = k.sb("lsm", [128, 8], F32)
        ltmp = k.sb("ltmp", [128, 64], F32)
        k.tt("dve", ltmp, lam[:, 0:64], lam[:, 64:128], ALU.mult)
        k.reduce("dve", lsm[:, 0:1], ltmp, ALU.add)
        k.tt("dve", ltmp, lam[:, 128:192], lam[:, 192:256], ALU.mult)
        k.reduce("dve", lsm[:, 1:2], ltmp, ALU.add)
        k.act(lsm[:, 2:4], lsm[:, 0:2], AF.Exp)
        k.tt("dve", lsm[:, 4:5], lsm[:, 3:4], lsm[:, 2:3], ALU.subtract)
        k.ts("dve", lsm[:, 5:6], lsm[:, 4:5], -lam_init, ALU.add)
        nlam = lsm[:, 5:6]
        sub = k.sb("subln", [128, 1], F32)
        k.dma("sp", sub, V(W["od_subln"].ap[i:i + 1, :].rearrange("o p -> p o"), W["od_subln"].bufs),
              allow_slow_non_contiguous=True)
        k.ts("dve", sub, sub, 1.0 - lam_init, ALU.mult)

        with k.scope():
            g.po_t = [k.ps("po%d" % j, [128, 512]) for j in range(2)]
            cosT = k.sb("cosT", [128, 2048], F32)
            sinT = k.sb("sinT", [128, 2048], F32)
            k.dma("sp", cosT, g.ropec)
            k.dma("sp", sinT, g.ropes)
            wq2 = k.sb("wq2", [128, 8, 128], BF16)
            wk2 = k.sb("wk2", [128, 8, 128], BF16)
            pp = [k.ps("pp%d" % j, [128, 512]) for j in range(2)]
            ps_ = [k.ps("pS%d" % j, [128, 512]) for j in range(2)]
            pnum = k.ps("pnum", [128, 512])
            pden = k.ps("pden", [128, 512])
            rt1 = k.sb("rt1", [128, 512], F32)
            rt2 = k.sb("rt2", [128, 512], F32)
            Eb = [k.sb("Eb%d" % j, [128, 512], BF16) for j in range(3)]
            Om = [k.sb("Om%d" % j, [128, 512], F32) for j in range(2)]
            rden = k.sb("rden", [128, 512], F32)
            Of = k.sb("Of", [128, 512], F32)
            sqb = k.sb("sqb", [128, 512], BF16)
            rsn = k.sb("rsn", [128, 512], F32)
            ecnt = 0
            for h in range(4):
                oc = oTc[h % 2]
                k.dma("pool", wq, V(win.ap[i, :, h * 128:(h + 1) * 128].rearrange("(kc p) c -> p kc c", p=128), win.bufs))
                k.dma("pool", wk, V(win.ap[i, :, 512 + h * 128:512 + (h + 1) * 128].rearrange("(kc p) c -> p kc c", p=128), win.bufs))
                k.dma("pool", wv, V(win.ap[i, :, 1024 + h * 128:1024 + (h + 1) * 128].rearrange("(kc p) c -> p kc c", p=128), win.bufs))
                for (ws, wd) in ((wq, wq2), (wk, wk2)):
                    for kc in range(8):
                        s4 = ws[:, kc, :].re("p (b t s) -> p b t s", b=4, t=2, s=16)
                        d4 = wd[:, kc, :].re("p (b t s) -> p b t s", b=4, t=2, s=16)
                        k.ts("pool", d4[:, :, 0, :], s4[:, :, 1, :], -1.0, ALU.mult)
                        k.copy("pool", d4[:, :, 1, :], s4[:, :, 0, :])
                nblk = 5
                for bi in range(nblk):
                    t0 = bi * 512
                    n = 512 if bi < 4 else 256
                    for (ws, wd, dst) in ((wq, wq2, QT), (wk, wk2, KT)):
                        if dst is QT and bi == 4 and not need_ctx:
                            continue
                        p1 = pp[0]
                        for kc in range(8):
                            k.mm(p1[:, 0:n], ws[:, kc, :], hTv(kc, t0, t0 + n), start=(kc == 0), stop=(kc == 7))
                        if bi < 4:
                            p2 = pp[1]
                            for kc in range(8):
                                k.mm(p2[:, 0:n], wd[:, kc, :], hTv(kc, t0, t0 + n), start=(kc == 0), stop=(kc == 7))
                            k.tt("dve", rt1, p1, cosT[:, t0:t0 + n], ALU.mult)
                            k.tt("dve", rt2, p2, sinT[:, t0:t0 + n], ALU.mult)
                            k.tt("pool", dst[:, t0:t0 + n], rt1, rt2, ALU.add)
                        else:
                            k.copy("act", dst[:, t0:t0 + n], p1[:, 0:n])
                for tt in range(NT):
                    p1 = pp[tt % 2]
                    for kc in range(8):
                        k.mm(p1[:, 0:128], hTv(kc, tt * 128, (tt + 1) * 128), wv[:, kc, :], start=(kc == 0), stop=(kc == 7))
                    k.copy("act", VV[:, tt, :], p1[:, 0:128])
                qblocks = [(b * 512, 512, list(range(NT))) for b in range(4)]
                if need_ctx:
                    qblocks.append((2048, 256, [16, 17]))
                for (q0, n, kts) in qblocks:
                    for m in range(2):
                        for ki, kt in enumerate(kts):
                            S = ps_[ecnt % 2]
                            E = Eb[ecnt % 3]
                            ecnt += 1
                            k.mm(S[:, 0:n], KT[m * 64:(m + 1) * 64, kt * 128:(kt + 1) * 128], QT[m * 64:(m + 1) * 64, q0:q0 + n])
                            k.act(E[:, 0:n], S[:, 0:n], AF.Exp, scale=0.125)
                            k.mm(pnum[:, 0:n], VV[:, kt, :], E[:, 0:n], start=(ki == 0), stop=(ki == len(kts) - 1))
                            k.mm(pden[:, 0:n], onesb, E[:, 0:n], start=(ki == 0), stop=(ki == len(kts) - 1))
                        k.recip(rden[:, 0:n], pden[:, 0:n])
                        k.tt("dve", Om[m][:, 0:n], pnum[:, 0:n], rden[:, 0:n], ALU.mult)
                    k.stt("dve", Of[:, 0:n], Om[1][:, 0:n], nlam, Om[0][:, 0:n], ALU.mult, ALU.add)
                    k.act(sqb[:, 0:n], Of[:, 0:n], AF.Square)
                    k.mm(pden[:, 0:n], onesb, sqb[:, 0:n])
                    k.rsqrt_from(rsn[:, 0:n], pden[:, 0:n], 1.0 / 128, 1e-5)
                    k.stt("dve", oc[:, q0:q0 + n], Of[:, 0:n], sub, rsn[:, 0:n], ALU.mult, ALU.mult)
                out_proj_chunk(g, W["od_w_out"], i, h * 128, oc, ntl, h)

        with k.scope():
            g.po_t = [k.ps("po%d" % j, [128, 512]) for j in range(2)]
            nmask = k.sb("nmask", [128, g.n_na_cls, 128], BF16)
            k.dma("pool", nmask, V(g.namask.ap.rearrange("c p q -> p c q"), g.namask.bufs))
            TB = k.sb("TB", [128, 16, 64], F32)
            EL = k.sb("EL", [128, 16, 896], BF16)
            EC = k.sb("EC", [128, 2, 2048], BF16)
            pp1 = k.ps("pp0", [128, 512])
            pp = [pp1, pp1]
            pS = [k.ps("pS%d" % j, [128, 1024]) for j in range(2)]
            pnd = k.ps("pnd", [128, 256])
            pnum = V(pnd.ap[:, 0:128], [Buf("pnum")])
            pden = V(pnd.ap[:, 128:256], [Buf("pden")])
            stmp = k.sb("stmp", [128, 896], F32)
            rden = k.sb("rden", [128, 128], F32)
            cnt = 0
            for hp in range(4):
                oc = oTc[hp % 2]
                c0 = 1536 + hp * 128
                k.dma("pool", wq, V(win.ap[i, :, c0:c0 + 128].rearrange("(kc p) c -> p kc c", p=128), win.bufs))
                k.dma("pool", wk, V(win.ap[i, :, 512 + c0:512 + c0 + 128].rearrange("(kc p) c -> p kc c", p=128), win.bufs))
                k.dma("pool", wv, V(win.ap[i, :, 1024 + c0:1024 + c0 + 128].rearrange("(kc p) c -> p kc c", p=128), win.bufs))
                for bi in range(5):
                    t0 = bi * 512
                    n = 512 if bi < 4 else 256
                    for (ws, dst) in ((wq, QT), (wk, KT)):
                        if dst is QT and bi == 4 and not need_ctx:
                            continue
                        p1 = pp[cnt % 2]
                        cnt += 1
                        for kc in range(8):
                            k.mm(p1[:, 0:n], ws[:, kc, :], hTv(kc, t0, t0 + n), start=(kc == 0), stop=(kc == 7))
                        k.copy("act", dst[:, t0:t0 + n], p1[:, 0:n])
                for tt in range(NT):
                    p1 = pp[cnt % 2]
                    cnt += 1
                    for kc in range(8):
                        k.mm(p1[:, 0:128], hTv(kc, tt * 128, (tt + 1) * 128), wv[:, kc, :], start=(kc == 0), stop=(kc == 7))
                    k.copy("act", VV[:, tt, :], p1[:, 0:128])
                for hh in range(2):
                    head = 2 * hp + hh
                    pb = hh * 64
                    k.dma("sp", TB, V(g.nabias.ap[i, head], g.nabias.bufs))
                    for kt in range(16):
                        j0 = max(0, kt - 3)
                        j1 = min(15, kt + 3)
                        nq = j1 - j0 + 1
                        S = pS[kt % 2]
                        c = 0
                        while c < nq * 128:
                            w = min(512, nq * 128 - c)
                            k.mm(S[:, c:c + w], KT[pb:pb + 64, kt * 128:(kt + 1) * 128],
                                 QT[pb:pb + 64, j0 * 128 + c:j0 * 128 + c + w])
                            c += w
                        u0 = 2 * (j0 - kt) + 7
                        k.stt("dve", stmp[:, 0:nq * 128], S[:, 0:nq * 128], 0.125,
                              TB[:, u0:u0 + 2 * nq, :].re("p u c -> p (u c)"), ALU.mult, ALU.add)
                        k.act(EL[:, kt, 0:nq * 128], stmp[:, 0:nq * 128], AF.Exp)
                        for j in range(j0, j1 + 1):
                            sl = EL[:, kt, (j - j0) * 128:(j - j0 + 1) * 128]
                            ci = g.na_cls.get((kt, j))
                            if ci is None:
                                continue
                            k.tt("pool", sl, sl, nmask[:, ci, :], ALU.mult)
                    for kc_ in range(2):
                        for qb in range(4):
                            S = pS[(kc_ * 4 + qb) % 2]
                            k.mm(S[:, 0:512], KT[pb:pb + 64, (16 + kc_) * 128:(17 + kc_) * 128], QT[pb:pb + 64, qb * 512:(qb + 1) * 512])
                            k.act(EC[:, kc_, qb * 512:(qb + 1) * 512], S[:, 0:512], AF.Exp, scale=0.125)
                    for j in range(16):
                        terms = []
                        for kt in range(max(0, j - 3), min(15, j + 3) + 1):
                            if g.na_cls.get((kt, j)) is None:
                                continue
                            j0 = max(0, kt - 3)
                            terms.append((kt, EL[:, kt, (j - j0) * 128:(j - j0 + 1) * 128]))
                        for kc_ in range(2):
                            terms.append((16 + kc_, EC[:, kc_, j * 128:(j + 1) * 128]))
                        for ti, (kt, Ev) in enumerate(terms):
                            k.mm(pnum, VV[:, kt, :], Ev, start=(ti == 0), stop=(ti == len(terms) - 1))
                        for ti, (kt, Ev) in enumerate(terms):
                            k.mm(pden, onesb, Ev, start=(ti == 0), stop=(ti == len(terms) - 1))
                        k.recip(rden, pden)
                        k.tt("dve", oc[pb:pb + 64, j * 128:(j + 1) * 128], pnum[pb:pb + 64, :], rden[pb:pb + 64, :], ALU.mult)
                    if need_ctx:
                        S = pS[0]
                        for kc_ in range(2):
                            k.mm(S[:, kc_ * 256:(kc_ + 1) * 256], KT[pb:pb + 64, (16 + kc_) * 128:(17 + kc_) * 128], QT[pb:pb + 64, 2048:2304])
                        k.act(EL[:, 0, 0:512], S[:, 0:512], AF.Exp, scale=0.125)
                        for half in range(2):
                            for kc_ in range(2):
                                k.mm(pnum, VV[:, 16 + kc_, :], EL[:, 0, kc_ * 256 + half * 128:kc_ * 256 + (half + 1) * 128],
                                     start=(kc_ == 0), stop=(kc_ == 1))
                            for kc_ in range(2):
                                k.mm(pden, onesb, EL[:, 0, kc_ * 256 + half * 128:kc_ * 256 + (half + 1) * 128],
                                     start=(kc_ == 0), stop=(kc_ == 1))
                            k.recip(rden, pden)
                            k.tt("dve", oc[pb:pb + 64, 2048 + half * 128:2048 + (half + 1) * 128], pnum[pb:pb + 64, :],
                                 rden[pb:pb + 64, :], ALU.mult)
                out_proj_chunk(g, W["od_w_out"], i, 512 + hp * 128, oc, ntl, hp)


def tri_tables():
    s = np.arange(64)[:, None]
    t = np.arange(64)[None, :]
    SU = (s < t).astype(np.float32)
    IU = (s <= t).astype(np.float32)
    SL = (s > t).astype(np.float32)
    IL = (s >= t).astype(np.float32)
    tri = np.stack([SU, IU, SL, IL, -IU, -IL], 0)
    c = np.float32(math.exp(-0.5))
    trif = np.stack([np.concatenate([IU, SU], 1), np.concatenate([IL, SL], 1)], 0) * (-c)
    return tri.astype(np.float32), trif.astype(np.float32)


def even_mixer(g, l, need_ctx):
    k = g.k
    W = g.W
    i = l // 2
    win = W["ev_w_in"]
    hTv = g.hTv
    id64 = g.ident[0:64, 0:64]

    def bc3(v, n):
        return v.re("p (o s) -> p o s", o=1).bc([64, n, 64])

    def bcl(v, n, m=64):
        return v.re("p (c o) -> p c o", o=1).bc([64, n, m])

    with k.scope():
        wo1 = k.sb("wo0", [128, 1024], BF16)
        g.wo_t = [wo1, wo1]
        g.otmp_t = [k.sb("otmp%d" % j, [128, 512], F32) for j in range(2)]
        g.po_t = [k.ps("po%d" % j, [128, 512]) for j in range(2)]
        oTc1 = k.sb("oTc0", [128, TOK], BF16)
        oTc = [oTc1, oTc1]
        pw = k.ps("pw", [128, 1024])
        pd = [k.ps("pd%d" % j, [128, 512]) for j in range(2)]
        pch = [k.ps("pch%d" % j, [128, 512]) for j in range(2)]
        g.pcv = []
        for d in range(2):
            bRU = Buf("pcRU%d" % d)
            bYA = Buf("pcYA%d" % d)
            g.pcv.append([V(pch[d].ap[0:64, 0:64], [bRU]), V(pch[d].ap[0:64, 64:128], [bRU]),
                          V(g.po_t[d].ap[0:64, 0:64], [bYA] + list(g.po_t[d].bufs)),
                          V(g.po_t[d].ap[0:64, 64:128], [bYA] + list(g.po_t[d].bufs))])

        import os as _os
        dbg = _os.environ.get("EVDBG", "")
        with k.scope():
            cw = k.sb("cw", [128, 4, 3], F32)
            for fc_ in range(4):
                k.dma("sp", cw[:, fc_, :], V(W["ev_conv_w"].ap[i, :, fc_ * 128:(fc_ + 1) * 128].rearrange("j p -> p j"), W["ev_conv_w"].bufs),
                      allow_slow_non_contiguous=True)
            wc = k.sb("wc", [128, 8, 384], BF16)
            ux = k.sb("ux", [128, 2050], F32)
            uc = k.sb("uc", [128, 258], F32)
            bg = k.sb("bg", [128, TOK], BF16)
            cx = k.sb("cx", [128, 512], F32)
            yc_ = k.sb("ycv", [128, 2048], F32)
            k.memset("dve", ux, 0.0)
            k.memset("dve", uc, 0.0)
            for fc in range(0 if "noconv" in dbg else 4):
                oc = oTc[fc % 2]
                for j in range(3):
                    c0 = 1920 + j * 512 + fc * 128
                    k.dma("pool", wc[:, :, j * 128:(j + 1) * 128],
                          V(win.ap[i, :, c0:c0 + 128].rearrange("(kc p) c -> p kc c", p=128), win.bufs))
                for bi in range(5):
                    t0 = bi * 512
                    n = 512 if bi < 4 else 256
                    pB, pC, pX = pd[0], pd[1], pch[0]
                    for (pp_, j) in ((pB, 0), (pC, 1), (pX, 2)):
                        for kc in range(8):
                            k.mm(pp_[:, 0:n], wc[:, kc, j * 128:(j + 1) * 128], hTv(kc, t0, t0 + n), start=(kc == 0), stop=(kc == 7))
                    k.copy("act", bg[:, t0:t0 + n], pB[:, 0:n])
                    k.copy("act", cx[:, 0:n], pC[:, 0:n])
                    dst = ux[:, 1 + t0:1 + t0 + n] if bi < 4 else uc[:, 1:257]
                    k.tt("dve", dst, cx[:, 0:n], pX[:, 0:n], ALU.mult)
                for (u, n, t0) in ((ux, 2048, 0), (uc, 256, 2048)):
                    y = yc_[:, 0:n]
                    k.ts("dve", y, u[:, 0:n], cw[:, fc, 0:1], ALU.mult)
                    k.stt("dve", y, u[:, 1:n + 1], cw[:, fc, 1:2], y, ALU.mult, ALU.add)
                    k.stt("dve", y, u[:, 2:n + 2], cw[:, fc, 2:3], y, ALU.mult, ALU.add)
                    k.tt("pool", oc[:, t0:t0 + n], y, bg[:, t0:t0 + n], ALU.mult)
                out_proj_chunk(g, W["ev_w_out"], i, 512 + fc * 128, oc, NT, fc)

        with k.scope():
            msk = k.sb("msk", [64, 6, 64], BF16)
            k.dma("pool", msk, V(g.tri.ap.rearrange("m s t -> s m t"), g.tri.bufs))
            trif = k.sb("trif", [64, 2, 128], F32)
            k.dma("sp", trif, V(g.trif.ap.rearrange("d s t -> s d t"), g.trif.bufs))
            ones64 = k.sb("ones64", [64, 64], BF16)
            k.memset("dve", ones64, 1.0)
            I8 = k.sb("I8", [64, 8, 64], BF16)
            for c_ in range(8):
                k.copy("pool", I8[:, c_, :], id64)
            wlo = k.sb("wlo", [128, 8, 384], BF16)
            k.dma("pool", wlo, V(win.ap[i, :, 1536:1920].rearrange("(kc p) c -> p kc c", p=128), win.bufs))
            wrkv = k.sb("wrkv", [128, 8, 192], BF16)
            wup = k.sb("wup", [64, 2, 64], BF16)
            aup = k.sb("aup", [64, 2, 64], BF16)
            gup = k.sb("gup", [128, 64], BF16)
            pv = k.sb("pvec", [64, 8], F32)
            rowv = k.sb("rowv", [64, 5, 64], F32)
            Yacc = k.sb("Yacc", [64, 36, 64], BF16)
            ot2 = k.sb("ot2", [64, 8, 128], BF16)
            k.memset("dve", ot2, 0.0)
            rTf = k.sb("rTf", [64, 512], BF16)
            krf = k.sb("krf", [64, 512], BF16)
            tA = k.sb("tA", [64, 512], F32)
            tB = k.sb("tB", [64, 512], F32)
            kkf = k.sb("kkf", [64, 512], F32)
            af = k.sb("af", [64, 512], BF16)
            sqb = k.sb("sqb", [64, 512], BF16)
            twT = k.sb("twT", [64, 512], BF16)
            adT = k.sb("adT", [64, 512], BF16)
            sg = k.sb("sg", [64, 8, 64], F32)
            Ep = k.sb("Ep", [64, 8, 64], F32)
            Em = k.sb("Em", [64, 8, 64], BF16)
            Ex = k.sb("Ex", [64, 8, 64], BF16)
            btT = k.sb("btT", [64, 8, 64], BF16)
            ktT = k.sb("ktT", [64, 8, 64], BF16)
            LbT = k.sb("LbT", [64, 8, 64], BF16)
            Lb = k.sb("Lb", [64, 8, 64], BF16)
            Xa = [k.sb("Xa%d" % j, [64, 8, 64], BF16) for j in range(2)]
            XTa = [k.sb("XTa%d" % j, [64, 8, 64], BF16) for j in range(2)]
            Fd = k.sb("Fd", [64, 8, 64], BF16)
            Yd = [k.sb("Yd%d" % j, [64, 8, 64], BF16) for j in range(2)]
            KR = [k.sb("KR%d" % d, [64, 8, 2, 64], BF16) for d in range(2)]
            LkT = [k.sb("LkT%d" % d, [64, 8, 64], BF16) for d in range(2)]
            MkT = [k.sb("MkT%d" % d, [64, 8, 64], BF16) for d in range(2)]
            MbTn = [k.sb("MbTn%d" % d, [64, 8, 64], BF16) for d in range(2)]
            TT = [k.sb("TT%d" % d, [64, 8, 64], BF16) for d in range(2)]
            ktok = [k.sb("ktok%d" % d, [64, 8, 64], BF16) for d in range(2)]
            btokn = [k.sb("btokn%d" % d, [64, 8, 64], BF16) for d in range(2)]
            Vt = [k.sb("Vt%d" % d, [64, 8, 64], BF16) for d in range(2)]
            PCt = [k.sb("PCt%d" % d, [64, 8], F32) for d in range(2)]
            Ast = [[k.sb("Ast%d_%d" % (d, j), [64, 64], BF16) for j in range(2)] for d in range(2)]
            Rsb = [k.sb("Rsb%d" % d, [64, 64], BF16) for d in range(2)]
            Usb = [k.sb("Usb%d" % d, [64, 64], BF16) for d in range(2)]
            rkvt = k.sb("rkvt", [64, 8, 192], BF16)
            sgT = k.sb("sgT", [128, 512], BF16)
            ycn = tA.re("p (c s) -> p c s", s=64)
            ysq = tB.re("p (c s) -> p c s", s=64)
            rsm = k.sb("rsm", [64, 4, 8], F32)

            MS_T = [msk[:, 0, :], msk[:, 2, :]]
            MI_T = [msk[:, 1, :], msk[:, 3, :]]
            nMI_T = [msk[:, 4, :], msk[:, 5, :]]
            MS = [msk[:, 2, :], msk[:, 0, :]]

            def vec_col(name, c0, j):
                src = W[name]
                k.dma("sp", pv[:, j:j + 1], V(src.ap[i:i + 1, c0:c0 + 64].rearrange("o p -> p o"), src.bufs),
                      allow_slow_non_contiguous=True)

            batches_f = [(2048, 4, 32), (0, 8, 0), (512, 8, 8), (1024, 8, 16), (1536, 8, 24)]
            batches_b = [(2048, 4, 32), (1536, 8, 24), (1024, 8, 16), (512, 8, 8), (0, 8, 0)]

            EVH = int(_os.environ.get("EVH", "8")); EVB = int(_os.environ.get("EVB", "5")); EVP = int(_os.environ.get("EVP", "99"))
            EVCHAIN = int(_os.environ.get("EVCHAIN", "1")); EVREAD = int(_os.environ.get("EVREAD", "1"))
            for h in range(0 if "norwkv" in dbg else EVH):
                oc = oTc[(h // 2) % 2]
                hoff = (h % 2) * 64
                c_r = h * 64
                for j, cbase in enumerate((0, 512, 1024)):
                    k.dma("pool", wrkv[:, :, j * 64:(j + 1) * 64],
                          V(win.ap[i, :, cbase + c_r:cbase + c_r + 64].rearrange("(kc p) c -> p kc c", p=128), win.bufs))
                for d in range(2):
                    k.dma("pool", wup[:, d, :], V(W["ev_decay_up"].ap[i, d, :, c_r:c_r + 64], W["ev_decay_up"].bufs))
                    k.dma("pool", aup[:, d, :], V(W["ev_iclr_up"].ap[i, d, :, c_r:c_r + 64], W["ev_iclr_up"].bufs))
                k.dma("pool", gup, V(W["ev_gate_up"].ap[i, :, c_r:c_r + 64], W["ev_gate_up"].bufs))
                vec_col("ev_k_k", c_r, 0)
                vec_col("ev_k_a", c_r, 1)
                for d in range(2):
                    src = W["ev_iclr_a0"]
                    k.dma("sp", pv[:, 3 + d:4 + d], V(src.ap[i, d:d + 1, c_r:c_r + 64].rearrange("o p -> p o"), src.bufs),
                          allow_slow_non_contiguous=True)
                    src = W["ev_decay_w0"]
                    k.dma("sp", rowv[:, d, :], V(src.ap[i, d:d + 1, c_r:c_r + 64].partition_broadcast(64), src.bufs))
                for j, nm in ((2, "ev_r_k"), (3, "ev_lnx_w"), (4, "ev_lnx_b")):
                    src = W[nm]
                    k.dma("sp", rowv[:, j, :], V(src.ap[i:i + 1, c_r:c_r + 64].partition_broadcast(64), src.bufs))
                k.ts("dve", pv[:, 2:3], pv[:, 1:2], -1.0, ALU.mult, 1.0, ALU.add)
                k.memset("dve", Yacc, 0.0)
                for d in range(2):
                    k.memset("dve", Ast[d][0], 0.0)
                stp = [0, 0]

                for bi in range(EVB):
                    for d in range(2):
                        t0, nch, cg0 = (batches_f if d == 0 else batches_b)[bi]
                        n = nch * 64
                        pq, pk_ = pd[0], pd[1]
                        if EVP < 1:
                            continue
                        for kc in range(8):
                            k.mm(pq[0:64, 0:n], wrkv[:, kc, 0:64], hTv(kc, t0, t0 + n), start=(kc == 0), stop=(kc == 7))
                        for kc in range(8):
                            k.mm(pk_[0:64, 0:n], wrkv[:, kc, 64:128], hTv(kc, t0, t0 + n), start=(kc == 0), stop=(kc == 7))
                        k.copy("act", rTf[:, 0:n], pq[0:64, 0:n])
                        k.copy("act", krf[:, 0:n], pk_[0:64, 0:n])
                        k.ts("dve", tA[:, 0:n], krf[:, 0:n], pv[:, 0:1], ALU.mult)
                        k.act(sqb[:, 0:n], tA[:, 0:n], AF.Square)
                        k.mm(pw[0:64, 0:n], ones64, sqb[:, 0:n])
                        k.ts("dve", tB[:, 0:n], pw[0:64, 0:n], 1e-24, ALU.max)
                        k.emit("act", lambda h_: h_.sqrt(out=tB[:, 0:n].ap, in_=tB[:, 0:n].ap), reads=[tB], writes=[tB])
                        k.recip(tB[:, 0:n], tB[:, 0:n])
                        k.tt("dve", kkf[:, 0:n], tA[:, 0:n], tB[:, 0:n], ALU.mult)
                        if EVP < 2:
                            continue
                        for kc in range(8):
                            k.mm(pq[0:64, 0:n], wlo[:, kc, d * 64:(d + 1) * 64], hTv(kc, t0, t0 + n), start=(kc == 0), stop=(kc == 7))
                        for kc in range(8):
                            k.mm(pk_[0:64, 0:n], wlo[:, kc, 128 + d * 64:128 + (d + 1) * 64], hTv(kc, t0, t0 + n), start=(kc == 0), stop=(kc == 7))
                        k.act(twT[:, 0:n], pq[0:64, 0:n], AF.Tanh)
                        k.copy("act", adT[:, 0:n], pk_[0:64, 0:n])
                        k.mm(pq[0:64, 0:n], aup[:, d, :], adT[:, 0:n])
                        k.act(af[:, 0:n], pq[0:64, 0:n], AF.Sigmoid, bias=pv[:, 3 + d:4 + d])
                        if EVP < 3:
                            continue
                        plw = V(pw.ap[0:64, 0:512].rearrange("p (c s) -> p c s", s=64), pw.bufs)
                        for c in range(nch):
                            k.mm(plw[:, c, :], twT[:, c * 64:(c + 1) * 64], wup[:, d, :])
                        k.tt("dve", sg[:, 0:nch, :], plw[:, 0:nch, :], bc3(rowv[:, d, :], nch), ALU.add)
                        k.act(sg[:, 0:nch, :], sg[:, 0:nch, :], AF.Sigmoid)
                        pcum = V(pw.ap[0:64, :].rearrange("p (c s) -> p c s", s=128), pw.bufs)
                        for c in range(nch):
                            k.mm(pcum[:, c, :], sg[:, c, :], trif[:, d, :])
                        k.act(Ep[:, 0:nch, :], pcum[:, 0:nch, 0:64], AF.Exp)
                        k.act(Em[:, 0:nch, :], pcum[:, 0:nch, 0:64], AF.Exp, scale=-1.0)
                        k.act(Ex[:, 0:nch, :], pcum[:, 0:nch, 64:128], AF.Exp)
                        tl = 63 if d == 0 else 0
                        k.copy("dve", PCt[d][:, 0:nch], Ep[:, 0:nch, tl])
                        r3 = lambda v: v[:, 0:n].re("p (c s) -> p c s", s=64)
                        k.tt("dve", KR[d][:, 0:nch, 0, :], r3(kkf), Ex[:, 0:nch, :], ALU.mult)
                        k.tt("pool", KR[d][:, 0:nch, 1, :], r3(rTf), Ep[:, 0:nch, :], ALU.mult)
                        k.tt("pool", tB[:, 0:n], af[:, 0:n], kkf[:, 0:n], ALU.mult)
                        k.tt("pool", btT[:, 0:nch, :], r3(tB), Em[:, 0:nch, :], ALU.mult)
                        k.ts("dve", tA[:, 0:n], af[:, 0:n], pv[:, 1:2], ALU.mult, pv[:, 2:3], ALU.add)
                        k.tt("dve", tA[:, 0:n], tA[:, 0:n], krf[:, 0:n], ALU.mult)
                        k.tt("dve", ktT[:, 0:nch, :], r3(tA), Em[:, 0:nch, :], ALU.mult)
                        if EVP < 4:
                            continue
                        pvv = V(pd[0].ap[0:64, :].rearrange("p (c s) -> p c s", s=64), pd[0].bufs)
                        for c in range(nch):
                            for kc in range(8):
                                k.mm(pvv[:, c, :], hTv(kc, t0 + c * 64, t0 + (c + 1) * 64), wrkv[:, kc, 128:192],
                                     start=(kc == 0), stop=(kc == 7))
                        k.copy("act", Vt[d][:, 0:nch, :], pvv[:, 0:nch, :])
                        if EVP < 5:
                            continue
                        for c in range(nch):
                            k.mm(pcum[:, c, :], btT[:, c, :], KR[d][:, c, :, :].re("p a s -> p (a s)"))
                        k.tt("dve", LbT[:, 0:nch, :], pcum[:, 0:nch, 0:64], bc3(MS_T[d], nch), ALU.mult)
                        k.tt("dve", MbTn[d][:, 0:nch, :], pcum[:, 0:nch, 64:128], bc3(nMI_T[d], nch), ALU.mult)
                        for c in range(nch):
                            k.mm(pcum[:, c, :], ktT[:, c, :], KR[d][:, c, :, :].re("p a s -> p (a s)"))
                        k.tt("dve", LkT[d][:, 0:nch, :], pcum[:, 0:nch, 0:64], bc3(MS_T[d], nch), ALU.mult)
                        k.tt("dve", MkT[d][:, 0:nch, :], pcum[:, 0:nch, 64:128], bc3(MI_T[d], nch), ALU.mult)
                        pl3 = V(pd[1].ap[0:64, :].rearrange("p (c s) -> p c s", s=64), pd[1].bufs)
                        for c in range(nch):
                            k.mm(pl3[:, c, :], KR[d][:, c, 0, :], btT[:, c, :])
                        k.tt("dve", Lb[:, 0:nch, :], pl3[:, 0:nch, :], bc3(MS[d], nch), ALU.mult)
                        if EVP < 6:
                            continue
                        ptr = V(pd[0].ap[0:64, :].rearrange("p (c s) -> p c s", s=64), pd[0].bufs)
                        for c in range(nch):
                            k.mm(ptr[:, c, :], ktT[:, c, :], id64)
                        k.copy("act", ktok[d][:, 0:nch, :], ptr[:, 0:nch, :])
                        for c in range(nch):
                            k.mm(ptr[:, c, :], btT[:, c, :], id64)
                        k.ts("dve", btokn[d][:, 0:nch, :], ptr[:, 0:nch, :], -1.0, ALU.mult)
                        if EVP < 7:
                            continue
                        X, XT = Lb, LbT
                        k.tt("pool", Yd[0][:, 0:nch, :], I8[:, 0:nch, :], LbT[:, 0:nch, :], ALU.subtract)
                        ycur = 0
                        for lev in range(int(_os.environ.get("EVLEV", "5"))):
                            pX = V(pd[0].ap[0:64, :].rearrange("p (c s) -> p c s", s=64), pd[0].bufs)
                            pXT = V(pd[1].ap[0:64, :].rearrange("p (c s) -> p c s", s=64), pd[1].bufs)
                            pY = V(pw.ap[0:64, 0:512].rearrange("p (c s) -> p c s", s=64), pw.bufs)
                            for c in range(nch):
                                k.mm(pX[:, c, :], XT[:, c, :], X[:, c, :])
                            if lev < 4:
                                for c in range(nch):
                                    k.mm(pXT[:, c, :], X[:, c, :], XT[:, c, :])
                            k.copy("act", Xa[lev % 2][:, 0:nch, :], pX[:, 0:nch, :])
                            if lev < 4:
                                k.copy("act", XTa[lev % 2][:, 0:nch, :], pXT[:, 0:nch, :])
                            for c in range(nch):
                                k.mm(pY[:, c, :], I8[:, c, :], Yd[ycur][:, c, :], start=True, stop=False)
                                k.mm(pY[:, c, :], Xa[lev % 2][:, c, :], Yd[ycur][:, c, :], start=False, stop=True)
                            if lev < 4:
                                k.copy("act", Yd[1 - ycur][:, 0:nch, :], pY[:, 0:nch, :])
                                ycur = 1 - ycur
                                X, XT = Xa[lev % 2], XTa[lev % 2]
                            else:
                                k.copy("act", TT[d][:, 0:nch, :], pY[:, 0:nch, :])
                    nchs = [batches_f[bi][1], batches_b[bi][1]]
                    for step in range(max(nchs) if EVCHAIN else 0):
                        for d in range(2):
                            t0, nch, cg0 = (batches_f if d == 0 else batches_b)[bi]
                            if step >= nch:
                                continue
                            c = step if d == 0 else nch - 1 - step
                            cg = cg0 + c
                            A = Ast[d][stp[d] % 2]
                            An = Ast[d][(stp[d] + 1) % 2]
                            stp[d] += 1
                            pc = pch[d]
                            pR = g.pcv[d][0]; pU = g.pcv[d][1]; pY_ = g.pcv[d][2]; pA = g.pcv[d][3]
                            k.mm(pR, KR[d][:, c, 0, :], A, start=True, stop=False)
                            k.mm(pR, LkT[d][:, c, :], Vt[d][:, c, :], start=False, stop=True)
                            k.copy("act", Rsb[d], pR)
                            k.mm(pU, TT[d][:, c, :], Rsb[d])
                            k.copy("act", Usb[d], pU)
                            k.mm(pY_, KR[d][:, c, 1, :], A, start=True, stop=False)
                            k.mm(pY_, MkT[d][:, c, :], Vt[d][:, c, :], start=False, stop=False)
                            k.mm(pY_, MbTn[d][:, c, :], Usb[d], start=False, stop=True)
                            k.mm(pA, id64, A, start=True, stop=False)
                            k.mm(pA, ktok[d][:, c, :], Vt[d][:, c, :], start=False, stop=False)
                            k.mm(pA, btokn[d][:, c, :], Usb[d], start=False, stop=True)
                            k.tt("dve", Yacc[:, cg, :], Yacc[:, cg, :], pY_, ALU.add)
                            k.ts("dve", An, pA, PCt[d][:, c:c + 1], ALU.mult)

                for (t0, nch, cg0) in (batches_f if EVREAD else []):
                    n = nch * 64
                    psg = pd[0]
                    for kc in range(8):
                        k.mm(psg[:, 0:n], wlo[:, kc, 256:384], hTv(kc, t0, t0 + n), start=(kc == 0), stop=(kc == 7))
                    k.act(sgT[:, 0:n], psg[:, 0:n], AF.Sigmoid)
                    prk = V(pw.ap[0:64, :].rearrange("p (c s) -> p c s", s=256), pw.bufs)
                    for half in range((nch + 3) // 4):
                        for c4 in range(4):
                            c = half * 4 + c4
                            for kc in range(8):
                                k.mm(prk[:, c4, 0:192], hTv(kc, t0 + c * 64, t0 + (c + 1) * 64), wrkv[:, kc, :],
                                     start=(kc == 0), stop=(kc == 7))
                        k.copy("act", rkvt[:, half * 4:half * 4 + 4, :], prk[:, 0:4, 0:192])
                    pg = V(pd[1].ap[0:64, :].rearrange("p (c s) -> p c s", s=64), pd[1].bufs)
                    for c in range(nch):
                        k.mm(pg[:, c, :], sgT[:, c * 64:(c + 1) * 64], gup)
                    y = Yacc[:, cg0:cg0 + nch, :]
                    s1, mu, s2, rk = (rsm[:, j, 0:nch] for j in range(4))
                    k.reduce("dve", s1, y, ALU.add)
                    k.ts("dve", mu, s1, 1.0 / 64, ALU.mult)
                    k.tt("dve", ycn[:, 0:nch, :], y, bcl(mu, nch), ALU.subtract)
                    k.act(ysq[:, 0:nch, :], ycn[:, 0:nch, :], AF.Square)
                    k.reduce("dve", s2, ysq[:, 0:nch, :], ALU.add)
                    k.rsqrt(s2, s2, 1.0 / 64, 64e-5)
                    k.tt("dve", ycn[:, 0:nch, :], ycn[:, 0:nch, :], bcl(s2, nch), ALU.mult)
                    k.tt("dve", ycn[:, 0:nch, :], ycn[:, 0:nch, :], bc3(rowv[:, 3, :], nch), ALU.mult)
                    k.tt("dve", ycn[:, 0:nch, :], ycn[:, 0:nch, :], bc3(rowv[:, 4, :], nch), ALU.add)
                    k.tt("pool", ysq[:, 0:nch, :], rkvt[:, 0:nch, 0:64], rkvt[:, 0:nch, 64:128], ALU.mult)
                    k.tt("pool", ysq[:, 0:nch, :], ysq[:, 0:nch, :], bc3(rowv[:, 2, :], nch), ALU.mult)
                    k.reduce("dve", rk, ysq[:, 0:nch, :], ALU.add)
                    k.tt("dve", ysq[:, 0:nch, :], rkvt[:, 0:nch, 128:192], bcl(rk, nch), ALU.mult)
                    k.tt("dve", ycn[:, 0:nch, :], ycn[:, 0:nch, :], ysq[:, 0:nch, :], ALU.add)
                    k.tt("dve", ot2[:, 0:nch, hoff:hoff + 64], ycn[:, 0:nch, :], pg[:, 0:nch, :], ALU.mult)
                    ptr2 = V(pd[0].ap[:, :].rearrange("p (c s) -> p c s", s=64), pd[0].bufs)
                    for c in range(nch):
                        k.mm(ptr2[:, c, :], ot2[:, c, :], id64)
                    k.copy("act", oc[hoff:hoff + 64, t0:t0 + n], ptr2[hoff:hoff + 64, 0:nch, :].re("p c s -> p (c s)"))
                if h % 2 == 1:
                    out_proj_chunk(g, W["ev_w_out"], i, (h // 2) * 128, oc, NT, h // 2)


_CACHE = {}


def _get_program(**kw):
    key = tuple(sorted(kw.items()))
    if key not in _CACHE:
        _CACHE[key] = build_program(**kw)
    return _CACHE[key]


def make_in_maps(inputs, cores):
    f = lambda a: np.ascontiguousarray(np.asarray(a, dtype=np.float32))
    shared = {}
    for name, shape in WSPECS:
        shared[name] = f(inputs[name]).reshape(shape)
    nam, _ = na_tables()
    shared["namask"] = nam
    shared["nabias"] = na_bias_layout(shared["od_rel_bias"])
    shared["ropec"], shared["ropes"] = rope_tables()
    shared["tri"], shared["trif"] = tri_tables()
    maps = []
    for b in cores:
        m = dict(shared)
        m["xin"] = np.ascontiguousarray(np.concatenate([f(inputs["x"][b]), f(inputs["ctx"][b])], axis=0))
        m["cc"] = np.ascontiguousarray(np.stack([f(inputs["c"][b]), f(inputs["c_ctx"])], axis=0))
        maps.append(m)
    return maps


def kernel(**inputs):
    k = _get_program()
    maps = make_in_maps(inputs, list(range(8)))
    res = run_bass_kernel_spmd(k.nc, maps, core_ids=list(range(8)))
    return np.stack([res.results[b]["out"] for b in range(8)], axis=0).astype(np.float32)
```
